# Optimizing a Trainium2 kernel written in Bass

```python
import math
import jax
import jax.numpy as jnp
from jax import lax
import numpy as np

D_MODEL = 1024
BATCH = 8
SEQ = 4096
DEPTH = 4

N_BRANCH = 5
BRANCH_W = D_MODEL // 4
HG_HEADS = 4
HG_DK = BRANCH_W // HG_HEADS
HG_CHUNK = 64
CONF_K = 31
SC_K = 3
AT_HEADS = 4
AT_KV_HEADS = 2
AT_HD = BRANCH_W // AT_HEADS
AT_BLOCK = 128
AT_WINDOW = 128
REL_BUCKETS = 32
REL_MAX_DIST = 128
MEM_LEN = 256
MEM_HEADS = 4
MEM_HD = BRANCH_W // MEM_HEADS
EPS = 1e-6

IN_COLS = (
    BRANCH_W, BRANCH_W, BRANCH_W, BRANCH_W, BRANCH_W,
    BRANCH_W, BRANCH_W, BRANCH_W,
    BRANCH_W, BRANCH_W, BRANCH_W, BRANCH_W,
    BRANCH_W, AT_KV_HEADS * AT_HD, AT_KV_HEADS * AT_HD, BRANCH_W,
    BRANCH_W, BRANCH_W,
)
IN_WIDTH = sum(IN_COLS)

kernel_name = 'hybrid_parallel_gated_encoder'

F32 = jnp.float32


def rmsnorm(x, g):
    xf = x.astype(F32)
    y = xf * lax.rsqrt(jnp.mean(xf * xf, axis=-1, keepdims=True) + EPS) * g.astype(F32)
    return y.astype(x.dtype)


def split_cols(u):
    offs = np.cumsum(IN_COLS)[:-1].tolist()
    return jnp.split(u, offs, axis=-1)


def depthwise_conv(x, w):
    k = w.shape[0]
    return lax.conv_general_dilated(
        x, w[:, None, :].astype(x.dtype), window_strides=(1,),
        padding=[((k - 1) // 2, (k - 1) // 2)],
        dimension_numbers=('NWC', 'WIO', 'NWC'),
        feature_group_count=x.shape[-1])


def gla_scan(q, k, v, log_f):
    B_, S_, H, dk = q.shape
    dv = v.shape[-1]
    nc = S_ // HG_CHUNK

    def chunks(t):
        return t.reshape(B_, nc, HG_CHUNK, H, t.shape[-1]).transpose(1, 0, 3, 2, 4)

    incl = jnp.tril(jnp.ones((HG_CHUNK, HG_CHUNK), bool))[..., None]

    def step(state, blk):
        qc, kc, vc, gc = blk
        a = jnp.cumsum(gc, axis=2)
        diff = a[:, :, :, None, :] - a[:, :, None, :, :]
        decay = jnp.exp(jnp.where(incl, diff, -jnp.inf))
        scores = jnp.einsum('bhtk,bhsk,bhtsk->bhts', qc, kc, decay)
        o = (jnp.einsum('bhts,bhsv->bhtv', scores, vc)
             + jnp.einsum('bhtk,bhkv->bhtv', qc * jnp.exp(a), state))
        a_end = a[:, :, -1:, :]
        state = (state * jnp.exp(a_end[:, :, 0, :, None])
                 + jnp.einsum('bhsk,bhsv->bhkv', kc * jnp.exp(a_end - a), vc))
        return state, o

    init = jnp.zeros((B_, H, dk, dv), F32)
    _, oc = lax.scan(step, init, (chunks(q), chunks(k), chunks(v), chunks(log_f)))
    return oc.transpose(1, 0, 3, 2, 4).reshape(B_, S_, H, dv)


def hgrn2_branch(q, f_fwd, f_bwd, i, z, lb, onorm):
    B_, S_, _ = q.shape

    def heads(t):
        return t.astype(F32).reshape(B_, S_, HG_HEADS, HG_DK)

    qh, ih = heads(q), heads(i)

    def direction(f_logit, lb_d):
        f = lb_d + (1.0 - lb_d) * jax.nn.sigmoid(f_logit.astype(F32))
        return heads(1.0 - f), heads(jnp.log(f))

    k_f, g_f = direction(f_fwd, lb[0])
    k_b, g_b = direction(f_bwd, lb[1])
    o_fwd = gla_scan(qh, k_f, ih, g_f)
    rev = lambda t: jnp.flip(t, axis=1)
    o_bwd = rev(gla_scan(rev(qh), rev(k_b), rev(ih), rev(g_b)))
    o = rmsnorm(o_fwd + o_bwd, onorm).reshape(B_, S_, BRANCH_W).astype(q.dtype)
    return o * jax.nn.silu(z)


def conformer_branch(a, b, z, w_dw, b_dw, ln_g, ln_b):
    u = a * jax.nn.sigmoid(b)
    c = (depthwise_conv(u, w_dw) + b_dw.astype(u.dtype)).astype(F32)
    mu = jnp.mean(c, axis=-1, keepdims=True)
    var = jnp.mean(jnp.square(c - mu), axis=-1, keepdims=True)
    n = (c - mu) * lax.rsqrt(var + EPS) * ln_g.astype(F32) + ln_b.astype(F32)
    return jax.nn.silu(n).astype(a.dtype) * jax.nn.silu(z)


def short_conv_branch(b, c, v, z, w):
    return b * depthwise_conv(c * v, w) * jax.nn.silu(z)


def t5_bucket(rel):
    half = REL_BUCKETS // 2
    n = -rel
    ret = jnp.where(n < 0, half, 0)
    n = jnp.abs(n)
    max_exact = half // 2
    large = max_exact + (jnp.log(jnp.maximum(n, 1).astype(F32) / max_exact)
                         / math.log(REL_MAX_DIST / max_exact)
                         * (half - max_exact)).astype(jnp.int32)
    large = jnp.minimum(large, half - 1)
    return ret + jnp.where(n < max_exact, n, large)


def banded_position_bias(rel_bias):
    qi = jnp.arange(AT_BLOCK)[:, None]
    sj = jnp.arange(3 * AT_BLOCK)[None, :]
    rel = (sj - AT_BLOCK) - qi
    return jnp.transpose(rel_bias[t5_bucket(rel)], (2, 0, 1)).astype(F32)


def window_attention(q, k, v, sink, pos_bias):
    B_, S_, _ = q.shape
    nb = S_ // AT_BLOCK
    G = AT_HEADS // AT_KV_HEADS
    qb = q.reshape(B_, nb, AT_BLOCK, AT_KV_HEADS, G, AT_HD)

    def band(t):
        tp = jnp.pad(t.reshape(B_, S_, AT_KV_HEADS, AT_HD),
                     ((0, 0), (AT_BLOCK, AT_BLOCK), (0, 0), (0, 0)))
        tp = tp.reshape(B_, nb + 2, AT_BLOCK, AT_KV_HEADS, AT_HD)
        return jnp.concatenate([tp[:, :-2], tp[:, 1:-1], tp[:, 2:]], axis=2)

    kb, vb = band(k), band(v)
    logits = jnp.einsum('bnqkgd,bnskd->bnkgqs', qb, kb).astype(F32) * AT_HD ** -0.5
    logits = logits + pos_bias.reshape(AT_KV_HEADS, G, AT_BLOCK, 3 * AT_BLOCK)
    qi = jnp.arange(AT_BLOCK)[None, :, None]
    sj = jnp.arange(3 * AT_BLOCK)[None, None, :]
    blk = jnp.arange(nb)[:, None, None]
    qpos = blk * AT_BLOCK + qi
    kpos = (blk - 1) * AT_BLOCK + sj
    valid = (jnp.abs(kpos - qpos) <= AT_WINDOW) & (kpos >= 0) & (kpos < S_)
    logits = jnp.where(valid[None, :, None, None], logits, -jnp.inf)
    sink_col = jnp.broadcast_to(sink.astype(F32).reshape(1, 1, AT_KV_HEADS, G, 1, 1),
                                logits.shape[:-1] + (1,))
    p = jax.nn.softmax(jnp.concatenate([logits, sink_col], axis=-1), axis=-1)[..., :-1]
    o = jnp.einsum('bnkgqs,bnskd->bnqkgd', p.astype(v.dtype), vb)
    return o.reshape(B_, S_, BRANCH_W)


def memory_cross_attention(q, mem_kv):
    B_, S_, _ = q.shape
    M = mem_kv.shape[1]
    k, v = jnp.split(mem_kv, 2, axis=-1)
    qh = q.reshape(B_, S_, MEM_HEADS, MEM_HD)
    kh = k.reshape(B_, M, MEM_HEADS, MEM_HD)
    vh = v.reshape(B_, M, MEM_HEADS, MEM_HD)
    logits = jnp.einsum('bshd,bmhd->bhsm', qh, kh).astype(F32) * MEM_HD ** -0.5
    p = jax.nn.softmax(logits, axis=-1).astype(v.dtype)
    return jnp.einsum('bhsm,bmhd->bshd', p, vh).reshape(B_, S_, BRANCH_W)


def setup_inputs(seed: int = 0) -> dict:
    key = jax.random.key(seed)
    ks = jax.random.split(key, 20)

    def nrm(k, shape, scale):
        return jax.random.normal(k, shape, F32) * scale

    W = BRANCH_W
    return {
        'x': nrm(ks[0], (BATCH, SEQ, D_MODEL), 1.0),
        'mem': nrm(ks[1], (BATCH, MEM_LEN, D_MODEL), 1.0),
        'norm_pre': 1.0 + nrm(ks[2], (DEPTH, D_MODEL), 0.05),
        'norm_post': 1.0 + nrm(ks[3], (DEPTH, D_MODEL), 0.05),
        'w_in': nrm(ks[4], (DEPTH, D_MODEL, IN_WIDTH), D_MODEL ** -0.5),
        'hg_lb_logits': nrm(ks[5], (DEPTH, 2, W), 0.1),
        'hg_onorm': 1.0 + nrm(ks[6], (DEPTH, HG_DK), 0.05),
        'conf_dw_w': nrm(ks[7], (DEPTH, CONF_K, W), CONF_K ** -0.5),
        'conf_dw_b': nrm(ks[8], (DEPTH, W), 0.02),
        'conf_ln_g': 1.0 + nrm(ks[9], (DEPTH, W), 0.05),
        'conf_ln_b': nrm(ks[10], (DEPTH, W), 0.02),
        'sc_w': nrm(ks[11], (DEPTH, SC_K, W), SC_K ** -0.5),
        'attn_sink': nrm(ks[12], (DEPTH, AT_HEADS), 0.5),
        'rel_bias': nrm(ks[13], (REL_BUCKETS, AT_HEADS), 0.5),
        'mem_norm': 1.0 + nrm(ks[14], (DEPTH, D_MODEL), 0.05),
        'w_mem_kv': nrm(ks[15], (DEPTH, D_MODEL, 2 * BRANCH_W), D_MODEL ** -0.5),
        'w_gate': nrm(ks[16], (DEPTH, N_BRANCH, D_MODEL, D_MODEL), D_MODEL ** -0.5),
        'w_branch': nrm(ks[17], (DEPTH, N_BRANCH, W, D_MODEL), W ** -0.5),
        'w_out': nrm(ks[18], (DEPTH, D_MODEL, D_MODEL), D_MODEL ** -0.5),
    }


def reference(x, mem, norm_pre, norm_post, w_in, hg_lb_logits, hg_onorm, conf_dw_w, conf_dw_b,
              conf_ln_g, conf_ln_b, sc_w, attn_sink, rel_bias, mem_norm, w_mem_kv, w_gate,
              w_branch, w_out):
    lb_soft = jax.nn.softmax(hg_lb_logits.astype(F32), axis=0)
    lower_bounds = jnp.cumsum(lb_soft, axis=0) - lb_soft[0]
    pos_bias = banded_position_bias(rel_bias)
    for l in range(DEPTH):
        h = rmsnorm(x, norm_pre[l])
        (hq, hf_f, hf_b, hi, hz, ca, cb, cz, sb, sc, sv, sz,
         aq, ak, av, az, mq, mz) = split_cols(h @ w_in[l])
        y_hg = hgrn2_branch(hq, hf_f, hf_b, hi, hz, lower_bounds[l], hg_onorm[l])
        y_cf = conformer_branch(ca, cb, cz, conf_dw_w[l], conf_dw_b[l], conf_ln_g[l], conf_ln_b[l])
        y_sc = short_conv_branch(sb, sc, sv, sz, sc_w[l])
        y_at = window_attention(aq, ak, av, attn_sink[l], pos_bias) * jax.nn.silu(az)
        mem_kv = rmsnorm(mem, mem_norm[l]) @ w_mem_kv[l]
        y_mem = memory_cross_attention(mq, mem_kv) * jax.nn.silu(mz)
        merged = None
        for n, yb in enumerate((y_hg, y_cf, y_sc, y_at, y_mem)):
            term = jax.nn.sigmoid(h @ w_gate[l, n]) * (yb @ w_branch[l, n])
            merged = term if merged is None else merged + term
        x = x + rmsnorm(merged @ w_out[l], norm_post[l])
    return x
```

```python
import contextlib
import numpy as np
import ml_dtypes
import concourse.bass as bass
import concourse.mybir as mybir
from concourse.bass_utils import run_bass_kernel_spmd

F32 = mybir.dt.float32
BF16 = mybir.dt.bfloat16
ALU = mybir.AluOpType
AF = mybir.ActivationFunctionType
AX = mybir.AxisListType

D = 1024
S = 4096
L = 4
W = 256
NTB = 8
TB = 512
INW = 4352
EPS = 1e-6
NPV = 98


class Res:
    __slots__ = ("name", "w", "r", "dkey")

    def __init__(self, name, dkey=None):
        self.name = name
        self.w = None
        self.r = {}
        self.dkey = dkey


class KB:
    def __init__(self, nc, es):
        self.nc = nc
        self.es = es
        self.eng = {"pe": nc.tensor, "dve": nc.vector, "act": nc.scalar, "pool": nc.gpsimd,
                    "sync": nc.sync}
        self.semh = {}
        self.cnt = {}
        for e in ("pe", "dve", "act", "pool"):
            self.semh[e] = es.enter_context(nc.semaphore("s_" + e))
            self.cnt[e] = 0
        self.seen = {e: {} for e in self.eng}
        self.ndk = 0
        self.occ = {}
        self.dcache = {}

    def new_epoch(self):
        self.occ = {}

    def dkey(self, name):
        i = self.occ.get(name, 0)
        self.occ[name] = i + 1
        ck = (name, i)
        if ck in self.dcache:
            return self.dcache[ck]
        k = "d%d_%s" % (self.ndk, name)
        self.ndk += 1
        self.semh[k] = self.es.enter_context(self.nc.semaphore(k))
        self.cnt[k] = 0
        self.dcache[ck] = k
        return k

    def res(self, name, dkey=None):
        return Res(name, dkey)

    def _need(self, need, ev):
        if ev is None:
            return
        k, v = ev
        if need.get(k, 0) < v:
            need[k] = v

    def _emit_waits(self, eng, need):
        for k, v in need.items():
            if k.startswith("d"):
                v = self.cnt[k]
            if self.seen[eng].get(k, 0) >= v:
                continue
            self.eng[eng].wait_ge(self.semh[k], v)
            self.seen[eng][k] = v

    def _deps(self, eng, reads, writes, acc):
        need = {}
        for r in reads:
            self._need(need, r.w)
        for w in writes:
            if not (acc and w.w is not None and w.w[0] == acc):
                self._need(need, w.w)
            for ev in w.r.values():
                self._need(need, ev)
        return need

    def op(self, eng, fn, reads=(), writes=(), acc=None):
        need = self._deps(eng, reads, writes, acc)
        self._emit_waits(eng, need)
        ins = fn(self.eng[eng])
        self.cnt[eng] += 1
        ins.then_inc(self.semh[eng], 1)
        ev = (eng, self.cnt[eng])
        for r in reads:
            r.r[eng] = ev
        for w in writes:
            w.w = ev
            w.r = {}
        return ins

    def dma(self, q, out, in_, reads, writes, nowaw=False):
        need = self._deps(q, reads, writes, None)
        if nowaw:
            for w in writes:
                if w.w is not None and w.w[0] == w.dkey and w.w[0] in need:
                    pass
        k = writes[0].dkey
        if self.cnt[k] > 0:
            need[k] = self.cnt[k]
        self._emit_waits(q, need)
        ins = self.eng[q].dma_start(out=out, in_=in_)
        self.cnt[k] += 16
        ins.then_inc(self.semh[k], 16)
        ev = (k, self.cnt[k])
        for r in reads:
            r.r[k] = ev
        for w in writes:
            w.w = ev
            w.r = {}
        return ins

    def barrier(self):
        need = {k: v for k, v in self.cnt.items() if v > 0}
        for e in ("pe", "dve", "act", "pool", "sync"):
            self._emit_waits(e, dict(need))


def _t5_bucket_table():
    import math
    rel = np.arange(-255, 256)
    half = 16
    n = -rel
    ret = np.where(n < 0, half, 0)
    n = np.abs(n)
    max_exact = half // 2
    large = max_exact + (np.log(np.maximum(n, 1).astype(np.float32) / max_exact)
                         / math.log(128 / max_exact) * (half - max_exact)).astype(np.int32)
    large = np.minimum(large, half - 1)
    b = ret + np.where(n < max_exact, n, large)
    return np.asarray(rel), np.asarray(b)


def host_consts():
    c = {}
    c["ident"] = np.eye(128, dtype=np.float32).astype(ml_dtypes.bfloat16)
    s = np.arange(64)[:, None]
    t = np.arange(64)[None, :]
    tri = np.stack([(s <= t), (s >= t)], axis=1).astype(np.float32)
    c["tri"] = tri.astype(ml_dtypes.bfloat16)
    bm = np.zeros((128, 128), np.float32)
    bm[:64, :64] = 1
    bm[64:, 64:] = 1
    c["blockmask"] = bm
    rel, b = _t5_bucket_table()
    sl = np.arange(128)[:, None, None]
    jj = np.arange(3)[None, :, None]
    qq = np.arange(128)[None, None, :]
    relm = (jj - 1) * 128 + sl - qq
    valid = np.abs(relm) <= 128
    bk = b[relm + 255]
    bm_ = np.zeros((128, 32, 3, 128), np.float32)
    for bb in range(32):
        bm_[:, bb] = (valid & (bk == bb))
    c["bmask"] = bm_.reshape(128, 32 * 384).astype(ml_dtypes.bfloat16)
    c["vmask"] = valid.astype(np.float32).reshape(128, 384)
    return c


def build(dbg=None, nlayers=L, stop=None, yT_ext=False, skipA=False, mixers=("sc", "conf", "mem", "attn", "hg"), hg_stop=None):
    nc = bass.Bass("TRN2", target_bir_lowering=False)

    def dram(name, shape, dt, kind="Internal"):
        return nc.dram_tensor(name, list(shape), dt, kind=kind)

    xT_t = dram("xT", [D, S], F32, "ExternalInput")
    memT_t = dram("memT", [D, 256], F32, "ExternalInput")
    win_t = dram("w_in", [L, D, INW], F32, "ExternalInput")
    wg_t = dram("w_gate", [L, 5, D, D], F32, "ExternalInput")
    wb_t = dram("w_branch", [L, 5, W, D], F32, "ExternalInput")
    wo_t = dram("w_out", [L, D, D], F32, "ExternalInput")
    wm_t = dram("w_mkv", [L, D, 512], F32, "ExternalInput")
    pv_t = dram("pv", [L, 128, NPV], F32, "ExternalInput")
    lbl_t = dram("lbl", [128, 16], F32, "ExternalInput")
    bro_t = dram("bro", [L, 68], F32, "ExternalInput")
    relb_t = dram("relb", [1, 128], F32, "ExternalInput")
    ident_t = dram("ident", [128, 128], BF16, "ExternalInput")
    tri_t = dram("tri", [64, 2, 64], BF16, "ExternalInput")
    bmask_t = dram("blockmask", [128, 128], F32, "ExternalInput")
    bkm_t = dram("bmask", [128, 32 * 384], BF16, "ExternalInput")
    vm_t = dram("vmask", [128, 384], F32, "ExternalInput")
    out_t = dram("out", [D, S], F32, "ExternalOutput")

    hT_t = dram("s_hT", [D, S], BF16)
    hgq_t = dram("s_hgq", [W, S], BF16)
    hff_t = dram("s_hff", [2, W, S], F32)
    cab_t = dram("s_cab", [2 * W, S], BF16)
    cz_t = dram("s_cz", [W, S], BF16)
    sbcv_t = dram("s_sbcv", [3 * W, S], BF16)
    sz_t = dram("s_sz", [W, S], BF16)
    aqT_t = dram("s_aqT", [W, S], BF16)
    akT_t = dram("s_akT", [128, S], BF16)
    mqT_t = dram("s_mqT", [W, S], BF16)
    hiz_t = dram("s_hiz", [S, 512], BF16)
    avz_t = dram("s_avz", [S, 384], BF16)
    mzz_t = dram("s_mzz", [S, 256], BF16)
    hqk_t = dram("s_hqk", [2, 2, W, S], BF16)
    yT_t = dram("s_yT", [5, W, S], BF16, "ExternalInput" if yT_ext else "Internal")
    mg_t = dram("s_mgT", [D, S], BF16)
    dbg_ts = {}
    if dbg:
        for nm, (shape, dt) in dbg.items():
            dbg_ts[nm] = dram("dbg_" + nm, shape, dt, "ExternalOutput")

    _sbc = [0]

    def SBT(name, shape, dt):
        _sbc[0] += 1
        return nc.sbuf_tensor("%s_u%d" % (name, _sbc[0]), shape, dt)

    es = contextlib.ExitStack()
    with es:
        kb = KB(nc, es)

        def sbt(name, shape, dt):
            return es.enter_context(SBT(name, list(shape), dt))

        def pst(name, shape, dt):
            return es.enter_context(nc.psum_tensor(name, list(shape), dt))

        def dres(name, n=NTB):
            k = kb.dkey(name)
            return [kb.res("%s%d" % (name, i), k) for i in range(n)]

        r_out = dres("out")
        r_hT = dres("hT")
        r_hgq = dres("hgq")
        r_hff = dres("hff")
        r_cab = dres("cab")
        r_cz = dres("cz")
        r_sbcv = dres("sbcv")
        r_sz = dres("sz")
        r_aqT = dres("aqT")
        r_akT = dres("akT")
        r_mqT = dres("mqT")
        r_hiz = dres("hiz")
        r_avz = dres("avz")
        r_mzz = dres("mzz")
        r_yT = [dres("yT%d" % n) for n in range(5)]
        r_mg = dres("mg")
        r_hqk = dres("hqk")
        r_dbg = dres("dbg", 1)[0]

        NPS = 4
        ps = [pst("ps%d" % i, [128, 512], F32) for i in range(NPS)]
        r_ps = [kb.res("ps%d" % i) for i in range(NPS)]
        pss = pst("pss", [128, 512], F32)
        r_pss = kb.res("pss")
        pacc = pst("pacc", [128, 512], F32)
        r_pacc = kb.res("pacc")
        pstb = [pst("pstb%d" % i, [128, 1024], BF16) for i in range(2)]
        r_pstb = [kb.res("pstb%d" % i) for i in range(2)]
        psi = [0]

        def next_ps():
            i = psi[0] % NPS
            psi[0] += 1
            return ps[i], r_ps[i]

        ident = sbt("ident_sb", [128, 128], BF16)
        r_ident = kb.res("ident", kb.dkey("ident"))
        ones = sbt("ones", [128, 128], F32)
        r_ones = kb.res("ones")
        pv = sbt("pv_sb", [128, L, NPV], F32)
        r_pv = kb.res("pv", kb.dkey("pv"))
        lbt = sbt("lbt", [128, 16], F32)
        omlb = sbt("omlb", [128, 16], F32)
        r_lb = kb.res("lb", kb.dkey("lb"))
        epsc = sbt("epsc", [128, 1], F32)
        r_eps = kb.res("eps")

        kb.dma("sync", ident[:], ident_t.ap(), [], [r_ident])
        kb.dma("sync", pv[:], pv_t.ap().rearrange("l p n -> p l n"), [], [r_pv])
        kb.op("dve", lambda e: e.memset(ones[:], 1.0), [], [r_ones])
        kb.op("dve", lambda e: e.memset(epsc[:], EPS), [], [r_eps])

        with contextlib.ExitStack() as es0:
            lbl = es0.enter_context(SBT("lbl_sb", [128, 16], F32))
            lsum = es0.enter_context(SBT("lsum", [128, 4], F32))
            kb.dma("sync", lbl[:], lbl_t.ap(), [], [r_lb])
            kb.op("act", lambda e: e.activation(out=lbl[:], in_=lbl[:], func=AF.Exp), [r_lb], [r_lb])
            lv = lbl[:].rearrange("p (l c) -> p l c", l=4)
            kb.op("dve", lambda e: e.tensor_tensor(out=lsum[:], in0=lv[:, 0, :], in1=lv[:, 1, :], op=ALU.add), [r_lb], [r_lb])
            kb.op("dve", lambda e: e.tensor_tensor(out=lsum[:], in0=lsum[:], in1=lv[:, 2, :], op=ALU.add), [r_lb], [r_lb])
            kb.op("dve", lambda e: e.tensor_tensor(out=lsum[:], in0=lsum[:], in1=lv[:, 3, :], op=ALU.add), [r_lb], [r_lb])
            kb.op("dve", lambda e: e.reciprocal(out=lsum[:], in_=lsum[:]), [r_lb], [r_lb])
            kb.op("dve", lambda e: e.tensor_tensor(out=lv, in0=lv, in1=lsum[:].unsqueeze(1).broadcast_to([128, 4, 4]), op=ALU.mult), [r_lb], [r_lb])
            lbv = lbt[:].rearrange("p (l c) -> p l c", l=4)
            kb.op("dve", lambda e: e.memset(lbv[:, 0, :], 0.0), [r_lb], [r_lb])
            for l in range(1, 4):
                kb.op("dve", lambda e, l=l: e.tensor_tensor(out=lbv[:, l, :], in0=lbv[:, l - 1, :], in1=lv[:, l, :], op=ALU.add), [r_lb], [r_lb])
            kb.op("dve", lambda e: e.tensor_scalar(out=omlb[:], in0=lbt[:], scalar1=-1.0, scalar2=1.0, op0=ALU.mult, op1=ALU.add), [r_lb], [r_lb])
            kb.barrier()

        memn = sbt("memn", [128, 8, 256], F32)
        r_memn = kb.res("memn", kb.dkey("memn"))
        expb = sbt("expb", [128, 4, 3, 128], BF16)
        r_expb = kb.res("expb")
        with contextlib.ExitStack() as es0:
            msq = es0.enter_context(SBT("msq", [128, 8, 256], F32))
            r_msq = kb.res("msq")
            mrs = es0.enter_context(SBT("mrs", [128, 256], F32))
            r_mrs = kb.res("mrs")
            kb.dma("sync", memn[:], memT_t.ap().rearrange("(k p) t -> p k t", p=128), [], [r_memn])
            kb.op("act", lambda e: e.activation(out=msq[:], in_=memn[:], func=AF.Square), [r_memn], [r_msq])
            for k in range(8):
                kb.op("pe", lambda e, k=k: e.matmul(pss[:, 0:256], lhsT=ones[:], rhs=msq[:, k, :], start=(k == 0), stop=(k == 7)), [r_ones, r_msq], [r_pss], acc="pe")
            kb.op("act", lambda e: e.activation(out=mrs[:], in_=pss[:, 0:256], func=AF.Sqrt, bias=epsc[:], scale=1.0 / D), [r_pss, r_eps], [r_mrs])
            kb.op("dve", lambda e: e.reciprocal(out=mrs[:], in_=mrs[:]), [r_mrs], [r_mrs])
            kb.op("dve", lambda e: e.tensor_tensor(out=memn[:], in0=memn[:], in1=mrs[:].unsqueeze(1).broadcast_to([128, 8, 256]), op=ALU.mult), [r_memn, r_mrs], [r_memn])
            rrow = es0.enter_context(SBT("rrow", [1, 128], F32))
            r_rrow = kb.res("rrow", kb.dkey("rrow"))
            rbc = es0.enter_context(SBT("rbc", [128, 128], F32))
            r_rbc = kb.res("rbc")
            bkm = es0.enter_context(SBT("bkm", [128, 32, 384], BF16))
            r_bkm = kb.res("bkm", kb.dkey("bkm"))
            vmk = es0.enter_context(SBT("vmk", [128, 384], F32))
            r_vmk = kb.res("vmk", kb.dkey("vmk"))
            bacc = es0.enter_context(SBT("bacc", [128, 4, 384], F32))
            r_bacc = kb.res("bacc")
            kb.dma("sync", rrow[:], relb_t.ap(), [], [r_rrow])
            kb.dma("sync", bkm[:], bkm_t.ap().rearrange("p (b n) -> p b n", b=32), [], [r_bkm])
            kb.dma("sync", vmk[:], vm_t.ap(), [], [r_vmk])
            kb.op("pe", lambda e: e.matmul(ps[0][:, 0:128], lhsT=ones[0:1, :], rhs=rrow[:], start=True, stop=True), [r_ones, r_rrow], [r_ps[0]])
            kb.op("dve", lambda e: e.tensor_copy(out=rbc[:], in_=ps[0][:, 0:128]), [r_ps[0]], [r_rbc])
            kb.op("dve", lambda e: e.memset(bacc[:], 0.0), [], [r_bacc])
            for h in range(4):
                for bb in range(32):
                    kb.op("dve", lambda e, h=h, bb=bb: e.scalar_tensor_tensor(out=bacc[:, h, :], in0=bkm[:, bb, :], scalar=rbc[:, bb * 4 + h:bb * 4 + h + 1], in1=bacc[:, h, :], op0=ALU.mult, op1=ALU.add),
                          [r_bkm, r_rbc, r_bacc], [r_bacc])
            kb.op("act", lambda e: e.activation(out=bacc[:], in_=bacc[:], func=AF.Exp), [r_bacc], [r_bacc])
            kb.op("dve", lambda e: e.tensor_tensor(out=expb[:].rearrange("p h j q -> p h (j q)"), in0=bacc[:], in1=vmk[:].unsqueeze(1).broadcast_to([128, 4, 384]), op=ALU.mult), [r_bacc, r_vmk], [r_expb])
            if dbg and "expb" in dbg:
                kb.dma("sync", dbg_ts["expb"].ap(), expb[:].rearrange("p h j q -> p (h j q)"), [r_expb], [r_dbg])
            kb.barrier()

        cast_rr = [0]

        def cast(out_ap, in_ap, reads, writes):
            e = ("dve", "act", "pool")[cast_rr[0] % 3]
            cast_rr[0] += 1
            if e == "act":
                kb.op("act", lambda en: en.copy(out=out_ap, in_=in_ap), reads, writes)
            else:
                kb.op(e, lambda en: en.tensor_copy(out=out_ap, in_=in_ap), reads, writes)

        evac_rr = [0]

        def evac(out_ap, in_ap, reads, writes, eng=None):
            if eng is None:
                eng = ("act", "dve")[evac_rr[0] % 2]
                evac_rr[0] += 1
            if eng == "act":
                kb.op("act", lambda en: en.copy(out=out_ap, in_=in_ap), reads, writes)
            else:
                kb.op("dve", lambda en: en.tensor_copy(out=out_ap, in_=in_ap), reads, writes)

        def load_w_bf16(esl, name, dst, src_fn, nk, ncols, r_dst, colblk=512):
            stg = [esl.enter_context(SBT("%s_stg%d" % (name, i), [128, nk, colblk], F32)) for i in range(2)]
            r_stg = [kb.res("%s_stg%d" % (name, i), kb.dkey(name + "stg")) for i in range(2)]
            i = 0
            for c0 in range(0, ncols, colblk):
                c1 = min(ncols, c0 + colblk)
                kb.dma("sync", stg[i % 2][:, :, 0:c1 - c0], src_fn(c0, c1), [], [r_stg[i % 2]])
                for k in range(nk):
                    cast(dst[:, k, c0:c1], stg[i % 2][:, k, 0:c1 - c0], [r_stg[i % 2]], [r_dst])
                i += 1

        def rms_rstd(esl, name, src, r_src, nk, ncol, ptile, r_ptile):
            sq = esl.enter_context(SBT(name + "_sq", [128, nk, ncol], F32))
            r_sq = kb.res(name + "_sq")
            rstd = esl.enter_context(SBT(name + "_rstd", [128, ncol], F32))
            r_rstd = kb.res(name + "_rstd")
            return sq, r_sq, rstd, r_rstd

        def phase_A(l):
            kb.new_epoch()
            src_t = xT_t if l == 0 else out_t
            with contextlib.ExitStack() as esl:
                wsb = esl.enter_context(SBT("A_w", [128, 8, INW], BF16))
                r_w = kb.res("A_w")
                with contextlib.ExitStack() as esw:
                    load_w_bf16(esw, "A", wsb, lambda c0, c1: win_t.ap()[l].rearrange("(k p) c -> p k c", p=128)[:, :, c0:c1], 8, INW, r_w)
                    kb.barrier()
                xt = [esl.enter_context(SBT("A_x%d" % i, [128, 8, TB], F32)) for i in range(2)]
                r_xt = [kb.res("A_x%d" % i, kb.dkey("A_x")) for i in range(2)]
                sq = esl.enter_context(SBT("A_sq", [128, 8, TB], F32))
                r_sq = kb.res("A_sq")
                rstd = esl.enter_context(SBT("A_rstd", [128, TB], F32))
                r_rstd = kb.res("A_rstd")
                hT = [esl.enter_context(SBT("A_h%d" % i, [128, 8, TB], BF16)) for i in range(2)]
                r_h = [kb.res("A_h%d" % i) for i in range(2)]
                NO = 3
                ob = [esl.enter_context(SBT("A_ob%d" % i, [128, 6, TB], BF16)) for i in range(NO)]
                r_ob = [kb.res("A_ob%d" % i) for i in range(NO)]
                of = [esl.enter_context(SBT("A_of%d" % i, [128, 2, TB], F32)) for i in range(2)]
                r_of = [kb.res("A_of%d" % i) for i in range(2)]
                oi = [0]
                ofi = [0]
                xv = src_t.ap().rearrange("(k p) t -> p k t", p=128)
                hv = hT_t.ap().rearrange("(k p) t -> p k t", p=128)
                kb.dma("sync", xt[0][:], xv[:, :, 0:TB], [r_out[0]], [r_xt[0]])
                for tb in range(NTB):
                    t0 = tb * TB
                    X = xt[tb % 2]
                    rX = r_xt[tb % 2]
                    if tb + 1 < NTB:
                        kb.dma("sync", xt[(tb + 1) % 2][:], xv[:, :, t0 + TB:t0 + 2 * TB], [r_out[tb + 1]], [r_xt[(tb + 1) % 2]])
                    kb.op("act", lambda e: e.activation(out=sq[:], in_=X[:], func=AF.Square), [rX], [r_sq])
                    for k in range(8):
                        kb.op("pe", lambda e, k=k: e.matmul(pss[:], lhsT=ones[:], rhs=sq[:, k, :], start=(k == 0), stop=(k == 7)),
                              [r_ones, r_sq], [r_pss], acc="pe")
                    kb.op("act", lambda e: e.activation(out=rstd[:], in_=pss[:], func=AF.Sqrt, bias=epsc[:], scale=1.0 / D), [r_pss, r_eps], [r_rstd])
                    kb.op("dve", lambda e: e.reciprocal(out=rstd[:], in_=rstd[:]), [r_rstd], [r_rstd])
                    H = hT[tb % 2]
                    rH = r_h[tb % 2]
                    for k in range(8):
                        kb.op("dve", lambda e, k=k: e.scalar_tensor_tensor(out=H[:, k, :], in0=X[:, k, :], scalar=pv[:, l, k:k + 1], in1=rstd[:], op0=ALU.mult, op1=ALU.mult),
                              [rX, r_pv, r_rstd], [rH])
                    kb.dma("pool", hv[:, :, t0:t0 + TB], H[:], [rH], [r_hT[tb]])

                    def fm_group(col0, nblk, dst_ap_fn, r_dst, f32=False):
                        if f32:
                            o = of[ofi[0] % 2]
                            ro = r_of[ofi[0] % 2]
                            ofi[0] += 1
                        else:
                            o = ob[oi[0] % NO]
                            ro = r_ob[oi[0] % NO]
                            oi[0] += 1
                        for b in range(nblk):
                            p, rp = next_ps()
                            for k in range(8):
                                kb.op("pe", lambda e, k=k, b=b: e.matmul(p[:], lhsT=wsb[:, k, col0 + b * 128: col0 + (b + 1) * 128], rhs=H[:, k, :], start=(k == 0), stop=(k == 7)),
                                      [r_w, rH], [rp], acc="pe")
                            evac(o[:, b, :], p[:], [rp], [ro])
                        kb.dma("pool", dst_ap_fn(t0), o[:, 0:nblk, :], [ro], [r_dst[tb]])

                    def tm_group(col0, ncol, dst_t, r_dst):
                        o = ob[oi[0] % NO]
                        ro = r_ob[oi[0] % NO]
                        oi[0] += 1
                        ov = o[:].rearrange("p a t -> p (a t)")[:, 0:4 * ncol].rearrange("p (s c) -> p s c", s=4)
                        for ts in range(4):
                            p, rp = next_ps()
                            for k in range(8):
                                kb.op("pe", lambda e, k=k, ts=ts: e.matmul(p[:, 0:ncol], lhsT=H[:, k, ts * 128:(ts + 1) * 128], rhs=wsb[:, k, col0:col0 + ncol], start=(k == 0), stop=(k == 7)),
                                      [r_w, rH], [rp], acc="pe")
                            evac(ov[:, ts, :], p[:, 0:ncol], [rp], [ro])
                        kb.dma("pool", dst_t.ap()[t0:t0 + TB, :].rearrange("(s p) c -> p s c", p=128), ov, [ro], [r_dst[tb]])

                    fmv = lambda t, r0, nb: (lambda tt: t.ap()[r0:r0 + nb * 128, :].rearrange("(b p) t -> p b t", p=128)[:, :, tt:tt + TB])
                    fm_group(0, 2, fmv(hgq_t, 0, 2), r_hgq)
                    fm_group(256, 2, lambda tt: hff_t.ap()[0].rearrange("(b p) t -> p b t", p=128)[:, :, tt:tt + TB], r_hff, f32=True)
                    fm_group(512, 2, lambda tt: hff_t.ap()[1].rearrange("(b p) t -> p b t", p=128)[:, :, tt:tt + TB], r_hff, f32=True)
                    tm_group(768, 512, hiz_t, r_hiz)
                    fm_group(1280, 4, fmv(cab_t, 0, 4), r_cab)
                    fm_group(1792, 2, fmv(cz_t, 0, 2), r_cz)
                    fm_group(2048, 6, fmv(sbcv_t, 0, 6), r_sbcv)
                    fm_group(2816, 2, fmv(sz_t, 0, 2), r_sz)
                    fm_group(3072, 2, fmv(aqT_t, 0, 2), r_aqT)
                    fm_group(3328, 1, fmv(akT_t, 0, 1), r_akT)
                    tm_group(3456, 384, avz_t, r_avz)
                    fm_group(3840, 2, fmv(mqT_t, 0, 2), r_mqT)
                    tm_group(4096, 256, mzz_t, r_mzz)
                kb.barrier()

        yTv = [yT_t.ap()[n].rearrange("(k p) t -> p k t", p=128) for n in range(5)]

        def build_diag(dst, fc, k, col):
            kb.op("dve", lambda e: e.tensor_scalar_mul(out=dst[:, fc, k, :], in0=ident[:], scalar1=col), [r_ident, r_pv], [r_dg])

        r_dg = kb.res("dg")

        def B_sc(l):
            kb.new_epoch()
            with contextlib.ExitStack() as esl:
                dg = esl.enter_context(SBT("sc_dg", [128, 2, 3, 128], BF16))
                for fc in range(2):
                    for k in range(3):
                        build_diag(dg, fc, k, pv[:, l, 92 + fc * 3 + k:93 + fc * 3 + k])
                tin = [esl.enter_context(SBT("sc_in%d" % i, [128, 6, TB + 2], BF16)) for i in range(2)]
                r_in = [kb.res("sc_in%d" % i, kb.dkey("sc_in")) for i in range(2)]
                tz = [esl.enter_context(SBT("sc_z%d" % i, [128, 2, TB], BF16)) for i in range(2)]
                r_z = [kb.res("sc_z%d" % i, kb.dkey("sc_z")) for i in range(2)]
                cv = esl.enter_context(SBT("sc_cv", [128, 2, TB + 2], BF16))
                r_cv = kb.res("sc_cv")
                sl = esl.enter_context(SBT("sc_sl", [128, 2, TB], BF16))
                r_sl = kb.res("sc_sl")
                yo = [esl.enter_context(SBT("sc_yo%d" % i, [128, 2, TB], BF16)) for i in range(2)]
                r_yo = [kb.res("sc_yo%d" % i) for i in range(2)]
                inv = sbcv_t.ap().rearrange("(b p) t -> p b t", p=128)
                zv = sz_t.ap().rearrange("(b p) t -> p b t", p=128)

                def loads(tb):
                    t0 = tb * TB
                    T = tin[tb % 2]
                    rT = r_in[tb % 2]
                    lo = max(t0 - 1, 0)
                    hi = min(t0 + TB + 1, S)
                    if tb == 0:
                        kb.op("dve", lambda e: e.memset(T[:, :, 0:1], 0.0), [], [rT])
                    if tb == NTB - 1:
                        kb.op("dve", lambda e: e.memset(T[:, :, TB + 1:TB + 2], 0.0), [], [rT])
                    kb.dma("sync", T[:, :, lo - (t0 - 1):hi - (t0 - 1)], inv[:, :, lo:hi], [r_sbcv[tb], r_sbcv[max(tb - 1, 0)], r_sbcv[min(tb + 1, NTB - 1)]], [rT])
                    kb.dma("sync", tz[tb % 2][:], zv[:, :, t0:t0 + TB], [r_sz[tb]], [r_z[tb % 2]])

                loads(0)
                for tb in range(NTB):
                    t0 = tb * TB
                    if tb + 1 < NTB:
                        loads(tb + 1)
                    T = tin[tb % 2]
                    rT = r_in[tb % 2]
                    kb.op("dve", lambda e: e.tensor_tensor(out=cv[:], in0=T[:, 2:4, :], in1=T[:, 4:6, :], op=ALU.mult), [rT], [r_cv])
                    kb.op("act", lambda e: e.activation(out=sl[:], in_=tz[tb % 2][:], func=AF.Silu), [r_z[tb % 2]], [r_sl])
                    kb.op("dve", lambda e: e.tensor_tensor(out=sl[:], in0=sl[:], in1=T[:, 0:2, 1:TB + 1], op=ALU.mult), [r_sl, rT], [r_sl])
                    Y = yo[tb % 2]
                    rY = r_yo[tb % 2]
                    for fc in range(2):
                        p, rp = next_ps()
                        for k in range(3):
                            kb.op("pe", lambda e, k=k, fc=fc: e.matmul(p[:], lhsT=dg[:, fc, k, :], rhs=cv[:, fc, k:k + TB], start=(k == 0), stop=(k == 2)),
                                  [r_dg, r_cv], [rp], acc="pe")
                        kb.op("dve", lambda e, fc=fc: e.tensor_tensor(out=Y[:, fc, :], in0=p[:], in1=sl[:, fc, :], op=ALU.mult), [rp, r_sl], [rY])
                    kb.dma("pool", yTv[2][:, :, t0:t0 + TB], Y[:], [rY], [r_yT[2][tb]])
                kb.barrier()

        def B_conf(l):
            kb.new_epoch()
            HL = 15
            with contextlib.ExitStack() as esl:
                dg = esl.enter_context(SBT("cf_dg", [128, 2, 31, 128], BF16))
                for fc in range(2):
                    for k in range(31):
                        build_diag(dg, fc, k, pv[:, l, 24 + fc * 31 + k:25 + fc * 31 + k])
                tin = [esl.enter_context(SBT("cf_in%d" % i, [128, 4, TB + 2 * HL], BF16)) for i in range(2)]
                r_in = [kb.res("cf_in%d" % i, kb.dkey("cf_in")) for i in range(2)]
                tz = [esl.enter_context(SBT("cf_z%d" % i, [128, 2, TB], BF16)) for i in range(2)]
                r_z = [kb.res("cf_z%d" % i, kb.dkey("cf_z")) for i in range(2)]
                u = esl.enter_context(SBT("cf_u", [128, 2, TB + 2 * HL], BF16))
                r_u = kb.res("cf_u")
                csb = esl.enter_context(SBT("cf_c", [128, 2, TB], F32))
                r_c = kb.res("cf_c")
                csq = esl.enter_context(SBT("cf_csq", [128, 2, TB], F32))
                r_csq = kb.res("cf_csq")
                mu = esl.enter_context(SBT("cf_mu", [128, TB], F32))
                r_mu = kb.res("cf_mu")
                var = esl.enter_context(SBT("cf_var", [128, TB], F32))
                r_var = kb.res("cf_var")
                sn = esl.enter_context(SBT("cf_sn", [128, 2, TB], BF16))
                r_sn = kb.res("cf_sn")
                sl = esl.enter_context(SBT("cf_sl", [128, 2, TB], BF16))
                r_sl = kb.res("cf_sl")
                yo = [esl.enter_context(SBT("cf_yo%d" % i, [128, 2, TB], BF16)) for i in range(2)]
                r_yo = [kb.res("cf_yo%d" % i) for i in range(2)]
                inv = cab_t.ap().rearrange("(b p) t -> p b t", p=128)
                zv = cz_t.ap().rearrange("(b p) t -> p b t", p=128)

                def loads(tb):
                    t0 = tb * TB
                    T = tin[tb % 2]
                    rT = r_in[tb % 2]
                    lo = max(t0 - HL, 0)
                    hi = min(t0 + TB + HL, S)
                    if tb == 0:
                        kb.op("dve", lambda e: e.memset(T[:, :, 0:HL], 0.0), [], [rT])
                    if tb == NTB - 1:
                        kb.op("dve", lambda e: e.memset(T[:, :, TB + HL:TB + 2 * HL], 0.0), [], [rT])
                    kb.dma("sync", T[:, :, lo - (t0 - HL):hi - (t0 - HL)], inv[:, :, lo:hi], [r_cab[tb], r_cab[max(tb - 1, 0)], r_cab[min(tb + 1, NTB - 1)]], [rT])
                    kb.dma("sync", tz[tb % 2][:], zv[:, :, t0:t0 + TB], [r_cz[tb]], [r_z[tb % 2]])

                loads(0)
                for tb in range(NTB):
                    t0 = tb * TB
                    if tb + 1 < NTB:
                        loads(tb + 1)
                    T = tin[tb % 2]
                    rT = r_in[tb % 2]
                    kb.op("act", lambda e: e.activation(out=u[:], in_=T[:, 2:4, :], func=AF.Sigmoid), [rT], [r_u])
                    kb.op("dve", lambda e: e.tensor_tensor(out=u[:], in0=u[:], in1=T[:, 0:2, :], op=ALU.mult), [r_u, rT], [r_u])
                    for fc in range(2):
                        p, rp = next_ps()
                        for k in range(31):
                            kb.op("pe", lambda e, k=k, fc=fc: e.matmul(p[:], lhsT=dg[:, fc, k, :], rhs=u[:, fc, k:k + TB], start=(k == 0), stop=(k == 30)),
                                  [r_dg, r_u], [rp], acc="pe")
                        kb.op("act", lambda e, fc=fc: e.activation(out=csb[:, fc, :], in_=p[:], func=AF.Identity, bias=pv[:, l, 86 + fc:87 + fc], scale=1.0), [rp, r_pv], [r_c])
                    kb.op("act", lambda e: e.activation(out=csq[:], in_=csb[:], func=AF.Square), [r_c], [r_csq])
                    for fc in range(2):
                        kb.op("pe", lambda e, fc=fc: e.matmul(pss[:], lhsT=ones[:], rhs=csb[:, fc, :], start=(fc == 0), stop=(fc == 1)), [r_ones, r_c], [r_pss], acc="pe")
                    for fc in range(2):
                        kb.op("pe", lambda e, fc=fc: e.matmul(pacc[:], lhsT=ones[:], rhs=csq[:, fc, :], start=(fc == 0), stop=(fc == 1)), [r_ones, r_csq], [r_pacc], acc="pe")
                    kb.op("dve", lambda e: e.tensor_scalar_mul(out=mu[:], in0=pss[:], scalar1=1.0 / W), [r_pss], [r_mu])
                    kb.op("dve", lambda e: e.tensor_tensor(out=var[:], in0=mu[:], in1=mu[:], op=ALU.mult), [r_mu], [r_var])
                    kb.op("dve", lambda e: e.scalar_tensor_tensor(out=var[:], in0=pacc[:], scalar=1.0 / W, in1=var[:], op0=ALU.mult, op1=ALU.subtract), [r_pacc, r_var], [r_var])
                    kb.op("act", lambda e: e.activation(out=var[:], in_=var[:], func=AF.Sqrt, bias=epsc[:], scale=1.0), [r_var, r_eps], [r_var])
                    kb.op("dve", lambda e: e.reciprocal(out=var[:], in_=var[:]), [r_var], [r_var])
                    Y = yo[tb % 2]
                    rY = r_yo[tb % 2]
                    kb.op("act", lambda e: e.activation(out=sl[:], in_=tz[tb % 2][:], func=AF.Silu), [r_z[tb % 2]], [r_sl])
                    for fc in range(2):
                        kb.op("dve", lambda e, fc=fc: e.tensor_tensor(out=csb[:, fc, :], in0=csb[:, fc, :], in1=mu[:], op=ALU.subtract), [r_c, r_mu], [r_c])
                        kb.op("dve", lambda e, fc=fc: e.tensor_tensor(out=csb[:, fc, :], in0=csb[:, fc, :], in1=var[:], op=ALU.mult), [r_c, r_var], [r_c])
                        kb.op("act", lambda e, fc=fc: e.activation(out=sn[:, fc, :], in_=csb[:, fc, :], func=AF.Silu, bias=pv[:, l, 90 + fc:91 + fc], scale=pv[:, l, 88 + fc:89 + fc]), [r_c, r_pv], [r_sn])
                    kb.op("dve", lambda e: e.tensor_tensor(out=Y[:], in0=sn[:], in1=sl[:], op=ALU.mult), [r_sn, r_sl], [rY])
                    kb.dma("pool", yTv[1][:, :, t0:t0 + TB], Y[:], [rY], [r_yT[1][tb]])
                kb.barrier()

        def finish_tm(esl, name):
            ya = esl.enter_context(SBT(name + "_ya", [128, 4, 64], F32))
            sl = esl.enter_context(SBT(name + "_sl", [128, 256], F32))
            ym = [esl.enter_context(SBT(name + "_ym%d" % i, [128, 256], BF16)) for i in range(2)]
            yo = [esl.enter_context(SBT(name + "_yo%d" % i, [128, 2, TB], BF16)) for i in range(2)]
            rd = esl.enter_context(SBT(name + "_rd", [128, 4], F32))
            return (ya, kb.res(name + "_ya"), sl, kb.res(name + "_sl"), ym, [kb.res(name + "_ym%d" % i) for i in range(2)],
                    yo, [kb.res(name + "_yo%d" % i) for i in range(2)], rd, kb.res(name + "_rd"))

        def tm_epilogue(F, idx, o_tile, r_o, z_ap, r_z, esink, r_es, q128, bi):
            ya, r_ya, sl, r_sl, ym, r_ym, yo, r_yo, rd, r_rd = F
            ov = o_tile[:, 0:260].rearrange("p (h e) -> p h e", e=65)
            if esink is not None:
                kb.op("dve", lambda e: e.tensor_tensor(out=rd[:], in0=ov[:, :, 64], in1=esink[:], op=ALU.add), [r_o, r_es], [r_rd])
            else:
                kb.op("dve", lambda e: e.tensor_copy(out=rd[:], in_=ov[:, :, 64]), [r_o], [r_rd])
            kb.op("dve", lambda e: e.reciprocal(out=rd[:], in_=rd[:]), [r_rd], [r_rd])
            kb.op("dve", lambda e: e.tensor_tensor(out=ya[:], in0=ov[:, :, 0:64], in1=rd[:].unsqueeze(2).broadcast_to([128, 4, 64]), op=ALU.mult), [r_o, r_rd], [r_ya])
            kb.op("act", lambda e: e.activation(out=sl[:], in_=z_ap, func=AF.Silu), [r_z], [r_sl])
            m = idx % 2
            kb.op("dve", lambda e: e.tensor_tensor(out=ym[m][:], in0=ya[:].rearrange("p h e -> p (h e)"), in1=sl[:], op=ALU.mult), [r_ya, r_sl], [r_ym[m]])
            if dbg and "eYA" in dbg and bi == 3 and q128 == 5:
                kb.dma("sync", dbg_ts["eYA"].ap(), ya[:].rearrange("p h e -> p (h e)"), [r_ya], [r_dbg])
                kb.dma("sync", dbg_ts["eSL"].ap(), sl[:], [r_sl], [r_dbg])
                kb.dma("sync", dbg_ts["eYM"].ap(), ym[m][:], [r_ym[m]], [r_dbg])
                kb.dma("sync", dbg_ts["eRD"].ap(), rd[:], [r_rd], [r_dbg])
            sub = q128 % 4
            for half in range(2):
                kb.op("pe", lambda e, half=half: e.transpose(out=pstb[half][:, sub * 128:(sub + 1) * 128], in_=ym[m][:, half * 128:(half + 1) * 128], identity=ident[:]),
                      [r_ym[m], r_ident], [r_pstb[half]], acc="pe")
            if sub == 3:
                tbi = q128 // 4
                Y = yo[tbi % 2]
                rY = r_yo[tbi % 2]
                for half in range(2):
                    evac(Y[:, half, :], pstb[half][:, 0:TB], [r_pstb[half]], [rY])
                if dbg and "eY" in dbg and bi == 3 and tbi == 1:
                    kb.dma("sync", dbg_ts["eY"].ap(), Y[:].rearrange("p k t -> p (k t)"), [rY], [r_dbg])
                kb.dma("pool", yTv[bi][:, :, tbi * TB:(tbi + 1) * TB], Y[:], [rY], [r_yT[bi][tbi]])

        def B_mem(l):
            kb.new_epoch()
            with contextlib.ExitStack() as esl:
                wm = esl.enter_context(SBT("m_w", [128, 8, 512], BF16))
                r_wm = kb.res("m_w")
                with contextlib.ExitStack() as esw:
                    load_w_bf16(esw, "M", wm, lambda c0, c1: wm_t.ap()[l].rearrange("(k p) c -> p k c", p=128)[:, :, c0:c1], 8, 512, r_wm)
                    kb.barrier()
                memh = esl.enter_context(SBT("m_h", [128, 8, 256], BF16))
                r_memh = kb.res("m_h")
                for k in range(8):
                    kb.op("dve", lambda e, k=k: e.tensor_scalar_mul(out=memh[:, k, :], in0=memn[:, k, :], scalar1=pv[:, l, 16 + k:17 + k]), [r_memn, r_pv], [r_memh])
                kT = esl.enter_context(SBT("m_kT", [128, 2, 256], BF16))
                r_kT = kb.res("m_kT")
                vaug = esl.enter_context(SBT("m_va", [128, 2, 4, 65], BF16))
                r_va = kb.res("m_va")
                kb.op("dve", lambda e: e.memset(vaug[:], 1.0), [], [r_va])
                for pair in range(2):
                    p, rp = next_ps()
                    for k in range(8):
                        kb.op("pe", lambda e, k=k, pair=pair: e.matmul(p[:, 0:256], lhsT=wm[:, k, pair * 128:(pair + 1) * 128], rhs=memh[:, k, :], start=(k == 0), stop=(k == 7)), [r_wm, r_memh], [rp], acc="pe")
                    evac(kT[:, pair, :], p[:, 0:256], [rp], [r_kT])
                for mb in range(2):
                    p, rp = next_ps()
                    for k in range(8):
                        kb.op("pe", lambda e, k=k, mb=mb: e.matmul(p[:, 0:256], lhsT=memh[:, k, mb * 128:(mb + 1) * 128], rhs=wm[:, k, 256:512], start=(k == 0), stop=(k == 7)), [r_wm, r_memh], [rp], acc="pe")
                    evac(vaug[:, mb, :, 0:64], p[:, 0:256].rearrange("p (h e) -> p h e", e=64), [rp], [r_va])
                tq = [esl.enter_context(SBT("m_q%d" % i, [128, 2, TB], BF16)) for i in range(2)]
                r_q = [kb.res("m_q%d" % i, kb.dkey("m_q")) for i in range(2)]
                tz = [esl.enter_context(SBT("m_z%d" % i, [128, 4, 256], BF16)) for i in range(2)]
                r_z = [kb.res("m_z%d" % i, kb.dkey("m_z")) for i in range(2)]
                pT = [esl.enter_context(SBT("m_pT%d" % i, [128, 2, TB], BF16)) for i in range(2)]
                r_pT = [kb.res("m_pT%d" % i) for i in range(2)]
                F = finish_tm(esl, "m")
                qv = mqT_t.ap().rearrange("(b p) t -> p b t", p=128)
                otl = [pss, pacc, ps[2], ps[3]]
                r_otl = [r_pss, r_pacc, r_ps[2], r_ps[3]]

                def loads(tb):
                    t0 = tb * TB
                    kb.dma("sync", tq[tb % 2][:], qv[:, :, t0:t0 + TB], [r_mqT[tb]], [r_q[tb % 2]])
                    kb.dma("sync", tz[tb % 2][:], mzz_t.ap()[t0:t0 + TB, :].rearrange("(s p) c -> p s c", p=128), [r_mzz[tb]], [r_z[tb % 2]])

                loads(0)
                ci = 0
                for tb in range(NTB):
                    if tb + 1 < NTB:
                        loads(tb + 1)
                    Q = tq[tb % 2]
                    rQ = r_q[tb % 2]
                    for h in range(4):
                        pair, base = h // 2, (h % 2) * 64
                        P = pT[ci % 2]
                        rP = r_pT[ci % 2]
                        ci += 1
                        for mb in range(2):
                            sc_, rsc = ps[mb], r_ps[mb]
                            kb.op("pe", lambda e, mb=mb: e.matmul(sc_[:], lhsT=kT[base:base + 64, pair, mb * 128:(mb + 1) * 128], rhs=Q[base:base + 64, pair, :], start=True, stop=True), [r_kT, rQ], [rsc])
                            kb.op("act", lambda e, mb=mb: e.activation(out=P[:, mb, :], in_=sc_[:], func=AF.Exp, scale=0.125), [rsc], [rP])
                        for ts in range(4):
                            ov = otl[ts][:, 0:260].rearrange("p (h e) -> p h e", e=65)
                            for mb in range(2):
                                kb.op("pe", lambda e, mb=mb, ts=ts: e.matmul(ov[:, h, :], lhsT=P[:, mb, ts * 128:(ts + 1) * 128], rhs=vaug[:, mb, h, :], start=(mb == 0), stop=(mb == 1)),
                                      [rP, r_va], [r_otl[ts]], acc=("pe" if (mb > 0 or h > 0) else None))
                    for ts in range(4):
                        tm_epilogue(F, tb * 4 + ts, otl[ts], r_otl[ts], tz[tb % 2][:, ts, :], r_z[tb % 2], None, None, tb * 4 + ts, 4)
                kb.barrier()

        def B_attn(l):
            kb.new_epoch()
            NB = 32
            with contextlib.ExitStack() as esl:
                akT = esl.enter_context(SBT("a_k", [128, S], BF16))
                r_ak = kb.res("a_k", kb.dkey("a_k"))
                aqT = esl.enter_context(SBT("a_q", [128, 2, S], BF16))
                r_aq = kb.res("a_q", kb.dkey("a_q"))
                vz = esl.enter_context(SBT("a_vz", [128, NB, 384], BF16))
                r_vz = kb.res("a_vz", kb.dkey("a_vz"))
                vaug = esl.enter_context(SBT("a_va", [128, NB, 2, 66], BF16))
                r_va = kb.res("a_va")
                esk = esl.enter_context(SBT("a_es", [128, 4], F32))
                r_es = kb.res("a_es")
                kb.dma("sync", akT[:], akT_t.ap(), r_akT, [r_ak])
                kb.dma("sync", aqT[:], aqT_t.ap().rearrange("(b p) t -> p b t", p=128), r_aqT, [r_aq])
                kb.dma("sync", vz[:], avz_t.ap().rearrange("(b p) c -> p b c", p=128), r_avz, [r_vz])
                srow = esl.enter_context(SBT("a_srow", [1, 4], F32))
                r_srow = kb.res("a_srow", kb.dkey("a_srow"))
                kb.dma("sync", srow[:], bro_t.ap()[l:l + 1, 64:68], [], [r_srow])
                kb.op("pe", lambda e: e.matmul(ps[0][:, 0:4], lhsT=ones[0:1, :], rhs=srow[:], start=True, stop=True), [r_ones, r_srow], [r_ps[0]])
                kb.op("act", lambda e: e.activation(out=esk[:], in_=ps[0][:, 0:4], func=AF.Exp), [r_ps[0]], [r_es])
                kb.op("dve", lambda e: e.memset(vaug[:], 1.0), [], [r_va])
                kb.op("dve", lambda e: e.tensor_copy(out=vaug[:, :, :, 0:64], in_=vz[:, :, 0:128].rearrange("p b (k d) -> p b k d", d=64)), [r_vz], [r_va])
                et = [esl.enter_context(SBT("a_e%d" % i, [128, 3, 128], BF16)) for i in range(2)]
                r_et = [kb.res("a_e%d" % i) for i in range(2)]
                pT = [esl.enter_context(SBT("a_p%d" % i, [128, 3, 128], BF16)) for i in range(2)]
                r_pT = [kb.res("a_p%d" % i) for i in range(2)]
                F = finish_tm(esl, "a")
                otl = [pss, pacc, ps[2], ps[3]]
                r_otl = [r_pss, r_pacc, r_ps[2], r_ps[3]]
                ci = 0
                for n in range(NB):
                    O = otl[n % 4]
                    rO = r_otl[n % 4]
                    ov = O[:, 0:260].rearrange("p (h e) -> p h e", e=65)
                    js = [j for j in range(3) if 0 <= n - 1 + j < NB]
                    for h in range(4):
                        chunk, base, kvh = h % 2, (h // 2) * 64, h // 2
                        sc_, rsc = ps[ci % 2], r_ps[ci % 2]
                        E, rE = et[ci % 2], r_et[ci % 2]
                        P, rP = pT[ci % 2], r_pT[ci % 2]
                        ci += 1
                        for j in js:
                            kb.op("pe", lambda e, j=j: e.matmul(sc_[:, j * 128:(j + 1) * 128], lhsT=akT[base:base + 64, (n - 1 + j) * 128:(n + j) * 128], rhs=aqT[base:base + 64, chunk, n * 128:(n + 1) * 128], start=True, stop=True),
                                  [r_ak, r_aq], [rsc], acc="pe")
                        j0, j1 = js[0], js[-1] + 1
                        kb.op("act", lambda e: e.activation(out=E[:, j0:j1, :], in_=sc_[:, j0 * 128:j1 * 128].rearrange("p (j q) -> p j q", q=128), func=AF.Exp, scale=0.125), [rsc], [rE])
                        kb.op("dve", lambda e: e.tensor_tensor(out=P[:, j0:j1, :], in0=E[:, j0:j1, :], in1=expb[:, h, j0:j1, :], op=ALU.mult), [rE, r_expb], [rP])
                        for j in js:
                            kb.op("pe", lambda e, j=j: e.matmul(ov[:, h, :], lhsT=P[:, j, :], rhs=vaug[:, n - 1 + j, kvh, 0:65], start=(j == js[0]), stop=(j == js[-1])),
                                  [rP, r_va], [rO], acc=("pe" if (j != js[0] or h > 0) else None))
                        if dbg and "aE" in dbg and n == 5 and h == 0:
                            kb.dma("sync", dbg_ts["aE"].ap(), E[:].rearrange("p j q -> p (j q)"), [rE], [r_dbg])
                            kb.dma("sync", dbg_ts["aP"].ap(), P[:].rearrange("p j q -> p (j q)"), [rP], [r_dbg])
                            dS = esl.enter_context(SBT("a_dS", [128, 384], F32))
                            r_dS = kb.res("a_dS")
                            kb.op("dve", lambda e: e.tensor_copy(out=dS[:], in_=sc_[:, 0:384]), [rsc], [r_dS])
                            kb.dma("sync", dbg_ts["aS"].ap(), dS[:], [r_dS], [r_dbg])
                            dO = esl.enter_context(SBT("a_dO", [128, 65], F32))
                            r_dO = kb.res("a_dO")
                            kb.op("dve", lambda e: e.tensor_copy(out=dO[:], in_=ov[:, 0, :]), [rO], [r_dO])
                            kb.dma("sync", dbg_ts["aO"].ap(), dO[:], [r_dO], [r_dbg])
                            kb.dma("sync", dbg_ts["aV"].ap().rearrange("p (j e) -> p j e", e=66), vaug[:, 4:7, 0, :], [r_va], [r_dbg])
                    tm_epilogue(F, n, O, rO, vz[:, n, 128:384], r_vz, esk, r_es, n, 3)
                kb.barrier()

        def B_hg(l):
            kb.new_epoch()
            GT = 1024
            NG1 = S // GT
            CG = GT // 64
            with contextlib.ExitStack() as esl:
                ev = esl.enter_context(SBT("h_ev", [128, 3, 2, 2, 64], F32))
                r_ev = kb.res("h_ev")
                with contextlib.ExitStack() as es1:
                    rmask = es1.enter_context(SBT("h_rm", [128, GT], BF16))
                    r_rm = kb.res("h_rm")
                    kb.op("dve", lambda e: e.memset(rmask[:], 1.0), [], [r_rm])
                    kb.op("dve", lambda e: e.memset(rmask[:].rearrange("p (c t) -> p c t", t=64)[:, :, 0:1], 0.0), [], [r_rm])
                    tq = [es1.enter_context(SBT("h_q%d" % i, [128, 2, GT], BF16)) for i in range(2)]
                    r_tq = [kb.res("h_q%d" % i, kb.dkey("h_q")) for i in range(2)]
                    tl = [es1.enter_context(SBT("h_l%d" % i, [128, GT], F32)) for i in range(2)]
                    r_tl = [kb.res("h_l%d" % i, kb.dkey("h_l")) for i in range(2)]
                    tg = es1.enter_context(SBT("h_g", [128, GT], F32))
                    r_tg = kb.res("h_g")
                    ta = es1.enter_context(SBT("h_a", [128, GT], F32))
                    r_ta = kb.res("h_a")
                    tb_ = es1.enter_context(SBT("h_b", [128, GT], F32))
                    r_tb = kb.res("h_b")
                    tc_ = es1.enter_context(SBT("h_c", [128, GT], F32))
                    r_tc = kb.res("h_c")
                    te = es1.enter_context(SBT("h_e", [128, GT], F32))
                    r_te = kb.res("h_e")
                    t16 = es1.enter_context(SBT("h_16", [128, CG], F32))
                    r_t16 = kb.res("h_16")
                    oq = [es1.enter_context(SBT("h_oq%d" % i, [128, GT], BF16)) for i in range(2)]
                    r_oq = [kb.res("h_oq%d" % i) for i in range(2)]
                    ok = [es1.enter_context(SBT("h_ok%d" % i, [128, GT], BF16)) for i in range(2)]
                    r_ok = [kb.res("h_ok%d" % i) for i in range(2)]
                    qv = hgq_t.ap().rearrange("(b p) t -> p b t", p=128)
                    it = 0
                    for g in range(NG1):
                        g0 = g * GT
                        rsrc = [r_hgq[2 * g], r_hgq[2 * g + 1]]
                        Q = tq[g % 2]
                        rQ = r_tq[g % 2]
                        kb.dma("sync", Q[:], qv[:, :, g0:g0 + GT], rsrc, [rQ])
                        for d in range(2):
                            for fc in range(2):
                                col = l * 4 + d * 2 + fc
                                TL = tl[it % 2]
                                rTL = r_tl[it % 2]
                                OQ, rOQ, OK, rOK = oq[it % 2], r_oq[it % 2], ok[it % 2], r_ok[it % 2]
                                it += 1
                                kb.dma("sync", TL[:], hff_t.ap()[d, fc * 128:(fc + 1) * 128, g0:g0 + GT], [r_hff[2 * g], r_hff[2 * g + 1]], [rTL])
                                kb.op("act", lambda e: e.activation(out=TL[:], in_=TL[:], func=AF.Sigmoid), [rTL], [rTL])
                                kb.op("dve", lambda e: e.tensor_scalar(out=TL[:], in0=TL[:], scalar1=omlb[:, col:col + 1], scalar2=lbt[:, col:col + 1], op0=ALU.mult, op1=ALU.add), [rTL, r_lb], [rTL])
                                kb.op("act", lambda e: e.activation(out=tg[:], in_=TL[:], func=AF.Ln), [rTL], [r_tg])
                                kb.op("dve", lambda e: e.tensor_tensor_scan(out=ta[:], data0=rmask[:], data1=tg[:], initial=0.0, op0=ALU.mult, op1=ALU.add), [r_rm, r_tg], [r_ta])
                                tav = ta[:].rearrange("p (c t) -> p c t", t=64)
                                if d == 0:
                                    A, rA = ta, r_ta
                                else:
                                    kb.op("dve", lambda e: e.tensor_tensor(out=tb_[:], in0=tg[:], in1=ta[:], op=ALU.subtract), [r_tg, r_ta], [r_tb])
                                    tbv = tb_[:].rearrange("p (c t) -> p c t", t=64)
                                    kb.op("dve", lambda e: e.tensor_tensor(out=tbv, in0=tbv, in1=tav[:, :, 63:64].broadcast_to([128, CG, 64]), op=ALU.add), [r_tb, r_ta], [r_tb])
                                    A, rA = tb_, r_tb
                                Av = A[:].rearrange("p (c t) -> p c t", t=64)
                                mid = Av[:, :, 32]
                                tot = Av[:, :, 63] if d == 0 else Av[:, :, 0]
                                c0 = g * CG
                                kb.op("act", lambda e: e.activation(out=ev[:, 0, d, fc, c0:c0 + CG], in_=mid, func=AF.Exp), [rA], [r_ev])
                                kb.op("act", lambda e: e.activation(out=ev[:, 1, d, fc, c0:c0 + CG], in_=tot, func=AF.Exp), [rA], [r_ev])
                                kb.op("dve", lambda e: e.tensor_tensor(out=t16[:], in0=tot, in1=mid, op=ALU.subtract), [rA], [r_t16])
                                kb.op("act", lambda e: e.activation(out=ev[:, 2, d, fc, c0:c0 + CG], in_=t16[:], func=AF.Exp), [r_t16], [r_ev])
                                tcv = tc_[:].rearrange("p (c t) -> p c t", t=64)
                                kb.op("dve", lambda e: e.tensor_tensor(out=tcv, in0=Av, in1=Av[:, :, 32:33].broadcast_to([128, CG, 64]), op=ALU.subtract), [rA], [r_tc])
                                kb.op("dve", lambda e: e.tensor_scalar(out=tc_[:], in0=tc_[:], scalar1=-40.0, scalar2=40.0, op0=ALU.max, op1=ALU.min), [r_tc], [r_tc])
                                kb.op("act", lambda e: e.activation(out=te[:], in_=tc_[:], func=AF.Exp), [r_tc], [r_te])
                                kb.op("dve", lambda e: e.tensor_tensor(out=OQ[:], in0=Q[:, fc, :], in1=te[:], op=ALU.mult), [rQ, r_te], [rOQ])
                                kb.op("act", lambda e: e.activation(out=te[:], in_=tc_[:], func=AF.Exp, scale=-1.0), [r_tc, r_te], [r_te])
                                kb.op("dve", lambda e: e.tensor_scalar(out=TL[:], in0=TL[:], scalar1=-1.0, scalar2=1.0, op0=ALU.mult, op1=ALU.add), [rTL], [rTL])
                                kb.op("dve", lambda e: e.tensor_tensor(out=OK[:], in0=TL[:], in1=te[:], op=ALU.mult), [rTL, r_te], [rOK])
                                kb.dma("pool", hqk_t.ap()[d, 0, fc * 128:(fc + 1) * 128, g0:g0 + GT], OQ[:], [rOQ], [r_hqk[2 * g]])
                                kb.dma("pool", hqk_t.ap()[d, 1, fc * 128:(fc + 1) * 128, g0:g0 + GT], OK[:], [rOK], [r_hqk[2 * g + 1]])
                    kb.barrier()
                if hg_stop == 1:
                    return
                Sp = esl.enter_context(SBT("h_Sp", [128, 2, 64, 2, 128], BF16))
                r_Sp = kb.res("h_Sp")
                hall = r_hqk + r_hiz
                with contextlib.ExitStack() as es2:
                    St = es2.enter_context(SBT("h_S", [128, 2, 2, 128], F32))
                    r_St = [[kb.res("h_S%d%d" % (d, fc)) for fc in range(2)] for d in range(2)]
                    bmk = es2.enter_context(SBT("h_bm", [128, 128], F32))
                    r_bmk = kb.res("h_bm", kb.dkey("h_bm"))
                    kb.dma("sync", bmk[:], bmask_t.ap(), [], [r_bmk])
                    for d in range(2):
                        for fc in range(2):
                            kb.op("dve", lambda e, d=d, fc=fc: e.memset(St[:, d, fc, :], 0.0), [], [r_St[d][fc]])
                    kt = [[es2.enter_context(SBT("h_kt%d%d" % (d, i), [128, 2, TB], BF16)) for i in range(2)] for d in range(2)]
                    r_kt = [[kb.res("h_kt%d%d" % (d, i), kb.dkey("h_kt")) for i in range(2)] for d in range(2)]
                    ti = [[es2.enter_context(SBT("h_i%d%d" % (d, i), [64, 8, 256], BF16)) for i in range(2)] for d in range(2)]
                    r_ti = [[kb.res("h_i%d%d" % (d, i), kb.dkey("h_i")) for i in range(2)] for d in range(2)]
                    ktm = [es2.enter_context(SBT("h_ktm%d" % d, [64, 2, 8, 128], BF16)) for d in range(2)]
                    r_ktm = [kb.res("h_ktm%d" % d) for d in range(2)]
                    t1 = [[es2.enter_context(SBT("h_t1%d%d" % (d, fc), [128, 128], F32)) for fc in range(2)] for d in range(2)]
                    r_t1 = [[kb.res("h_t1%d%d" % (d, fc)) for fc in range(2)] for d in range(2)]

                    def p1_load(d, step):
                        grp = step if d == 0 else NTB - 1 - step
                        t0 = grp * TB
                        kb.dma("sync", kt[d][step % 2][:], hqk_t.ap()[d, 1].rearrange("(b p) t -> p b t", p=128)[:, :, t0:t0 + TB], hall, [r_kt[d][step % 2]])
                        kb.dma("sync", ti[d][step % 2][:], hiz_t.ap()[t0:t0 + TB, 0:256].rearrange("(c p) v -> p c v", p=64), hall, [r_ti[d][step % 2]])

                    for d in range(2):
                        p1_load(d, 0)
                    for step in range(NTB):
                        for d in range(2):
                            if step + 1 < NTB:
                                p1_load(d, step + 1)
                            grp = step if d == 0 else NTB - 1 - step
                            KT, rKT = kt[d][step % 2], r_kt[d][step % 2]
                            TI, rTI = ti[d][step % 2], r_ti[d][step % 2]
                            for fc in range(2):
                                for c in range(8):
                                    kb.op("pe", lambda e, fc=fc, c=c: e.transpose(out=pstb[fc][0:64, c * 128:(c + 1) * 128], in_=KT[:, fc, c * 64:(c + 1) * 64], identity=ident[:]),
                                          [rKT, r_ident], [r_pstb[fc]], acc="pe")
                                evac(ktm[d][:, fc, :, :], pstb[fc][0:64, :].rearrange("p (c k) -> p c k", k=128), [r_pstb[fc]], [r_ktm[d]])
                            corder = range(8) if d == 0 else range(7, -1, -1)
                            for c in corder:
                                cg = grp * 8 + c
                                for fc in range(2):
                                    p, rp = next_ps()
                                    kb.op("pe", lambda e, fc=fc, c=c: e.matmul(p[:, 0:128], lhsT=ktm[d][:, fc, c, :], rhs=TI[:, c, fc * 128:(fc + 1) * 128], start=True, stop=True),
                                          [r_ktm[d], rTI], [rp])
                                    kb.op("dve", lambda e, fc=fc: e.scalar_tensor_tensor(out=t1[d][fc][:], in0=p[:, 0:128], scalar=ev[:, 2, d, fc, cg:cg + 1], in1=bmk[:], op0=ALU.mult, op1=ALU.mult),
                                          [rp, r_ev, r_bmk], [r_t1[d][fc]])
                                    kb.op("dve", lambda e, fc=fc: e.tensor_scalar_mul(out=Sp[:, d, cg, fc, :], in0=St[:, d, fc, :], scalar1=ev[:, 0, d, fc, cg:cg + 1]),
                                          [r_St[d][fc], r_ev], [r_Sp])
                                    kb.op("dve", lambda e, fc=fc: e.scalar_tensor_tensor(out=St[:, d, fc, :], in0=St[:, d, fc, :], scalar=ev[:, 1, d, fc, cg:cg + 1], in1=t1[d][fc][:], op0=ALU.mult, op1=ALU.add),
                                          [r_St[d][fc], r_ev, r_t1[d][fc]], [r_St[d][fc]])
                    kb.barrier()
                if hg_stop == 2:
                    return
                with contextlib.ExitStack() as es3:
                    trit = es3.enter_context(SBT("h_tri", [64, 2, 64], BF16))
                    r_trit = kb.res("h_tri", kb.dkey("h_tri"))
                    kb.dma("sync", trit[:], tri_t.ap(), [], [r_trit])
                    orow = es3.enter_context(SBT("h_orow", [1, 64], F32))
                    r_orow = kb.res("h_orow", kb.dkey("h_orow"))
                    onb = es3.enter_context(SBT("h_onb", [64, 64], F32))
                    r_onb = kb.res("h_onb")
                    kb.dma("sync", orow[:], bro_t.ap()[l:l + 1, 0:64], [], [r_orow])
                    kb.op("pe", lambda e: e.matmul(ps[0][0:64, 0:64], lhsT=ones[0:1, 0:64], rhs=orow[:], start=True, stop=True), [r_ones, r_orow], [r_ps[0]])
                    kb.op("dve", lambda e: e.tensor_copy(out=onb[:], in_=ps[0][0:64, 0:64]), [r_ps[0]], [r_onb])
                    if hg_stop == 3:
                        kb.barrier()
                        return
                    qk = [es3.enter_context(SBT("h_qk%d" % i, [128, 2, 2, 2, TB], BF16)) for i in range(2)]
                    r_qk = [kb.res("h_qk%d" % i, kb.dkey("h_qk")) for i in range(2)]
                    tiz = [es3.enter_context(SBT("h_iz%d" % i, [64, 8, 512], BF16)) for i in range(2)]
                    r_tiz = [kb.res("h_iz%d" % i, kb.dkey("h_iz")) for i in range(2)]
                    Pm = [es3.enter_context(SBT("h_pm%d" % i, [64, 2, 4, 64], BF16)) for i in range(2)]
                    r_Pm = [kb.res("h_pm%d" % i) for i in range(2)]
                    osb = es3.enter_context(SBT("h_osb", [64, 8, 256], F32))
                    r_osb = kb.res("h_osb")
                    osq = es3.enter_context(SBT("h_osq", [64, 8, 256], F32))
                    r_osq = kb.res("h_osq")
                    ssq = es3.enter_context(SBT("h_ssq", [64, 32], F32))
                    r_ssq = kb.res("h_ssq")
                    ym = es3.enter_context(SBT("h_ym", [64, 8, 256], BF16))
                    r_ym = kb.res("h_ym")
                    yo = [es3.enter_context(SBT("h_yo%d" % i, [128, 2, TB], BF16)) for i in range(2)]
                    r_yo = [kb.res("h_yo%d" % i) for i in range(2)]

                    def p2_load(g):
                        t0 = g * TB
                        for d in range(2):
                            for x in range(2):
                                kb.dma("sync", qk[g % 2][:, d, x, :, :], hqk_t.ap()[d, x].rearrange("(b p) t -> p b t", p=128)[:, :, t0:t0 + TB], hall, [r_qk[g % 2]])
                        kb.dma("sync", tiz[g % 2][:], hiz_t.ap()[t0:t0 + TB, :].rearrange("(c p) v -> p c v", p=64), hall, [r_tiz[g % 2]])

                    p2_load(0)
                    ci = 0
                    for g in range(NTB):
                        if g + 1 < NTB:
                            p2_load(g + 1)
                        QK, rQK = qk[g % 2], r_qk[g % 2]
                        IZ, rIZ = tiz[g % 2], r_tiz[g % 2]
                        for c in range(8):
                            cg = g * 8 + c
                            op_, rop = ps[2 + ci % 2], r_ps[2 + ci % 2]
                            PM, rPM = Pm[ci % 2], r_Pm[ci % 2]
                            ci += 1
                            for hl in range(2):
                                for d in range(2):
                                    for fc in range(2):
                                        kb.op("pe", lambda e, d=d, fc=fc, hl=hl: e.matmul(ps[hl][0:64, (d * 2 + fc) * 64:(d * 2 + fc + 1) * 64], lhsT=QK[hl * 64:(hl + 1) * 64, d, 1, fc, c * 64:(c + 1) * 64],
                                                                                         rhs=QK[hl * 64:(hl + 1) * 64, d, 0, fc, c * 64:(c + 1) * 64], start=True, stop=True),
                                              [rQK], [r_ps[hl]], acc="pe")
                            if hg_stop == 6:
                                continue
                            for hl in range(2):
                                kb.op("dve", lambda e, hl=hl: e.tensor_tensor(out=PM[:, :, hl::2, :], in0=ps[hl][0:64, 0:256].rearrange("p (d f t) -> p d f t", d=2, f=2), in1=trit[:].unsqueeze(2).broadcast_to([64, 2, 2, 64]), op=ALU.mult),
                                      [r_ps[hl], r_trit], [rPM])
                            if hg_stop == 7:
                                continue
                            for h in range(4):
                                fc, hl = h // 2, h % 2
                                oo = op_[0:64, h * 64:(h + 1) * 64]
                                kb.op("pe", lambda e, h=h: e.matmul(oo, lhsT=PM[:, 0, h, :], rhs=IZ[:, c, h * 64:(h + 1) * 64], start=True, stop=False), [rPM, rIZ], [rop], acc=("pe" if h > 0 else None))
                                kb.op("pe", lambda e, h=h: e.matmul(oo, lhsT=PM[:, 1, h, :], rhs=IZ[:, c, h * 64:(h + 1) * 64], start=False, stop=False), [rPM, rIZ], [rop], acc="pe")
                                for d in range(2):
                                    kb.op("pe", lambda e, d=d, fc=fc, hl=hl: e.matmul(oo, lhsT=QK[:, d, 0, fc, c * 64:(c + 1) * 64], rhs=Sp[:, d, cg, fc, hl * 64:(hl + 1) * 64], start=False, stop=(d == 1)),
                                          [rQK, r_Sp], [rop], acc="pe")
                            kb.op("act", lambda e: e.copy(out=osb[:, c, :], in_=op_[0:64, 0:256]), [rop], [r_osb])
                        if hg_stop in (4, 6, 7, 8):
                            continue
                        kb.op("act", lambda e: e.activation(out=osq[:], in_=osb[:], func=AF.Square), [r_osb], [r_osq])
                        kb.op("dve", lambda e: e.tensor_reduce(out=ssq[:], in_=osq[:].rearrange("p c (h v) -> p (c h) v", v=64), axis=AX.X, op=ALU.add), [r_osq], [r_ssq])
                        kb.op("act", lambda e: e.activation(out=ssq[:], in_=ssq[:], func=AF.Sqrt, bias=epsc[0:64, :], scale=1.0 / 64), [r_ssq, r_eps], [r_ssq])
                        kb.op("dve", lambda e: e.reciprocal(out=ssq[:], in_=ssq[:]), [r_ssq], [r_ssq])
                        ov3 = osb[:].rearrange("p c (h v) -> p (c h) v", v=64)
                        kb.op("dve", lambda e: e.tensor_tensor(out=ov3, in0=ov3, in1=ssq[:].unsqueeze(2).broadcast_to([64, 32, 64]), op=ALU.mult), [r_osb, r_ssq], [r_osb])
                        kb.op("dve", lambda e: e.tensor_tensor(out=ov3, in0=ov3, in1=onb[:].unsqueeze(1).broadcast_to([64, 32, 64]), op=ALU.mult), [r_osb, r_onb], [r_osb])
                        kb.op("act", lambda e: e.activation(out=osq[:], in_=IZ[:, :, 256:512], func=AF.Silu), [rIZ, r_osq], [r_osq])
                        kb.op("dve", lambda e: e.tensor_tensor(out=ym[:], in0=osb[:], in1=osq[:], op=ALU.mult), [r_osb, r_osq], [r_ym])
                        if hg_stop == 5:
                            continue
                        Y, rY = yo[g % 2], r_yo[g % 2]
                        for half in range(2):
                            for c in range(8):
                                kb.op("pe", lambda e, half=half, c=c: e.transpose(out=pstb[half][:, c * 64:(c + 1) * 64], in_=ym[:, c, half * 128:(half + 1) * 128], identity=ident[0:64, 0:64]),
                                      [r_ym, r_ident], [r_pstb[half]], acc="pe")
                            evac(Y[:, half, :], pstb[half][:, 0:TB], [r_pstb[half]], [rY])
                        kb.dma("pool", yTv[0][:, :, g * TB:(g + 1) * TB], Y[:], [rY], [r_yT[0][g]])
                    kb.barrier()

        def phase_B(l):
            if "sc" in mixers:
                B_sc(l)
            if "conf" in mixers:
                B_conf(l)
            if "mem" in mixers:
                B_mem(l)
            if "attn" in mixers:
                B_attn(l)
            if "hg" in mixers:
                B_hg(l)

        def phase_C(l):
            kb.new_epoch()
            with contextlib.ExitStack() as esl:
                hT = esl.enter_context(SBT("C_h", [128, 8, S], BF16))
                r_h = kb.res("C_h", kb.dkey("C_h"))
                yT = esl.enter_context(SBT("C_y", [128, 10, S], BF16))
                r_y = kb.res("C_y", kb.dkey("C_y"))
                hv = hT_t.ap().rearrange("(k p) t -> p k t", p=128)
                for k in range(8):
                    kb.dma("sync", hT[:, k, :], hv[:, k, :], r_hT, [r_h])
                yv = yT_t.ap().rearrange("n (k p) t -> p (n k) t", p=128)
                for n in range(5):
                    kb.dma("sync", yT[:, 2 * n:2 * n + 2, :], yv[:, 2 * n:2 * n + 2, :], r_yT[n], [r_y])
                wgb = [esl.enter_context(SBT("C_wg%d" % i, [128, 5, 8, 128], BF16)) for i in range(2)]
                wbb = [esl.enter_context(SBT("C_wb%d" % i, [128, 5, 2, 128], BF16)) for i in range(2)]
                r_wd = [kb.res("C_w%d" % i) for i in range(2)]
                sg = [esl.enter_context(SBT("C_sg%d" % i, [128, TB], BF16)) for i in range(3)]
                r_sg = [kb.res("C_sg%d" % i) for i in range(3)]
                tn = [esl.enter_context(SBT("C_tn%d" % i, [128, TB], BF16)) for i in range(3)]
                r_tn = [kb.res("C_tn%d" % i) for i in range(3)]
                mo = [esl.enter_context(SBT("C_mo%d" % i, [128, TB], BF16)) for i in range(2)]
                r_mo = [kb.res("C_mo%d" % i) for i in range(2)]
                stg_g = [esl.enter_context(SBT("C_sgt%d" % i, [128, 8, 128], F32)) for i in range(2)]
                r_stg_g = [kb.res("C_sgt%d" % i, kb.dkey("C_sgt")) for i in range(2)]
                stg_b = [esl.enter_context(SBT("C_sbt%d" % i, [128, 2, 128], F32)) for i in range(2)]
                r_stg_b = [kb.res("C_sbt%d" % i, kb.dkey("C_sbt")) for i in range(2)]
                si = [0]

                def load_d(d):
                    wg = wgb[d % 2]
                    wb = wbb[d % 2]
                    rw = r_wd[d % 2]
                    for n in range(5):
                        i = si[0] % 2
                        si[0] += 1
                        kb.dma("sync", stg_g[i][:], wg_t.ap()[l, n].rearrange("(k p) c -> p k c", p=128)[:, :, d * 128:(d + 1) * 128], [], [r_stg_g[i]])
                        kb.dma("sync", stg_b[i][:], wb_t.ap()[l, n].rearrange("(k p) c -> p k c", p=128)[:, :, d * 128:(d + 1) * 128], [], [r_stg_b[i]])
                        cast(wg[:, n, :, :], stg_g[i][:], [r_stg_g[i]], [rw])
                        cast(wb[:, n, :, :], stg_b[i][:], [r_stg_b[i]], [rw])

                load_d(0)
                cnt = 0
                mgv = mg_t.ap().rearrange("(k p) t -> p k t", p=128)
                for d in range(8):
                    if d + 1 < 8:
                        load_d(d + 1)
                    wg = wgb[d % 2]
                    wb = wbb[d % 2]
                    rw = r_wd[d % 2]
                    for tb in range(NTB):
                        t0 = tb * TB
                        for n in range(5):
                            pG, rG = next_ps()
                            for k in range(8):
                                kb.op("pe", lambda e, k=k, n=n: e.matmul(pG[:], lhsT=wg[:, n, k, :], rhs=hT[:, k, t0:t0 + TB], start=(k == 0), stop=(k == 7)),
                                      [rw, r_h], [rG], acc="pe")
                            j = cnt % 3
                            cnt += 1
                            kb.op("act", lambda e, j=j: e.activation(out=sg[j][:], in_=pG[:], func=AF.Sigmoid), [rG], [r_sg[j]])
                            pP, rP = next_ps()
                            for k in range(2):
                                kb.op("pe", lambda e, k=k, n=n: e.matmul(pP[:], lhsT=wb[:, n, k, :], rhs=yT[:, 2 * n + k, t0:t0 + TB], start=(k == 0), stop=(k == 1)),
                                      [rw, r_y], [rP], acc="pe")
                            kb.op("dve", lambda e, j=j: e.tensor_tensor(out=tn[j][:], in0=pP[:], in1=sg[j][:], op=ALU.mult), [rP, r_sg[j]], [r_tn[j]])
                            kb.op("pe", lambda e, j=j, n=n: e.matmul(pacc[:], lhsT=ident[:], rhs=tn[j][:], start=(n == 0), stop=(n == 4)),
                                  [r_ident, r_tn[j]], [r_pacc], acc=("pe" if n > 0 else None))
                        m = (d * NTB + tb) % 2
                        evac(mo[m][:], pacc[:], [r_pacc], [r_mo[m]])
                        kb.dma("pool", mgv[:, d, t0:t0 + TB], mo[m][:], [r_mo[m]], [r_mg[tb]])
                kb.barrier()

        def phase_D(l):
            kb.new_epoch()
            src_t = xT_t if l == 0 else out_t
            with contextlib.ExitStack() as esl:
                wsb = esl.enter_context(SBT("D_w", [128, 8, D], BF16))
                r_w = kb.res("D_w")
                with contextlib.ExitStack() as esw:
                    load_w_bf16(esw, "D", wsb, lambda c0, c1: wo_t.ap()[l].rearrange("(k p) c -> p k c", p=128)[:, :, c0:c1], 8, D, r_w)
                    kb.barrier()
                xt = [esl.enter_context(SBT("D_x%d" % i, [128, 8, TB], F32)) for i in range(2)]
                r_xt = [kb.res("D_x%d" % i, kb.dkey("D_x")) for i in range(2)]
                mt = [esl.enter_context(SBT("D_m%d" % i, [128, 8, TB], BF16)) for i in range(2)]
                r_mt = [kb.res("D_m%d" % i, kb.dkey("D_m")) for i in range(2)]
                o = esl.enter_context(SBT("D_o", [128, 8, TB], F32))
                r_o = kb.res("D_o")
                sq = esl.enter_context(SBT("D_sq", [128, 8, TB], F32))
                r_sq = kb.res("D_sq")
                rstd = esl.enter_context(SBT("D_rstd", [128, TB], F32))
                r_rstd = kb.res("D_rstd")
                xv = src_t.ap().rearrange("(k p) t -> p k t", p=128)
                ov = out_t.ap().rearrange("(k p) t -> p k t", p=128)
                mgv = mg_t.ap().rearrange("(k p) t -> p k t", p=128)

                def loads(tb):
                    t0 = tb * TB
                    kb.dma("sync", xt[tb % 2][:], xv[:, :, t0:t0 + TB], [r_out[tb]], [r_xt[tb % 2]])
                    kb.dma("sync", mt[tb % 2][:], mgv[:, :, t0:t0 + TB], [r_mg[tb]], [r_mt[tb % 2]])

                loads(0)
                for tb in range(NTB):
                    t0 = tb * TB
                    if tb + 1 < NTB:
                        loads(tb + 1)
                    X = xt[tb % 2]
                    rX = r_xt[tb % 2]
                    M = mt[tb % 2]
                    rM = r_mt[tb % 2]
                    for do in range(8):
                        p, rp = next_ps()
                        for k in range(8):
                            kb.op("pe", lambda e, k=k, do=do: e.matmul(p[:], lhsT=wsb[:, k, do * 128:(do + 1) * 128], rhs=M[:, k, :], start=(k == 0), stop=(k == 7)),
                                  [r_w, rM], [rp], acc="pe")
                        evac(o[:, do, :], p[:], [rp], [r_o])
                    kb.op("act", lambda e: e.activation(out=sq[:], in_=o[:], func=AF.Square), [r_o], [r_sq])
                    for k in range(8):
                        kb.op("pe", lambda e, k=k: e.matmul(pss[:], lhsT=ones[:], rhs=sq[:, k, :], start=(k == 0), stop=(k == 7)),
                              [r_ones, r_sq], [r_pss], acc="pe")
                    kb.op("act", lambda e: e.activation(out=rstd[:], in_=pss[:], func=AF.Sqrt, bias=epsc[:], scale=1.0 / D), [r_pss, r_eps], [r_rstd])
                    kb.op("dve", lambda e: e.reciprocal(out=rstd[:], in_=rstd[:]), [r_rstd], [r_rstd])
                    for k in range(8):
                        kb.op("dve", lambda e, k=k: e.scalar_tensor_tensor(out=o[:, k, :], in0=o[:, k, :], scalar=pv[:, l, 8 + k:9 + k], in1=rstd[:], op0=ALU.mult, op1=ALU.mult),
                              [r_o, r_pv, r_rstd], [r_o])
                    kb.op("pool", lambda e: e.tensor_tensor(out=X[:], in0=X[:], in1=o[:], op=ALU.add), [rX, r_o], [rX])
                    kb.dma("pool", ov[:, :, t0:t0 + TB], X[:], [rX], [r_out[tb]])
                kb.barrier()

        def dbg_out(name, src_ap_dram, reads):
            kb.dma("sync", dbg_ts[name].ap(), src_ap_dram, reads, [r_dbg])

        for l in range(nlayers):
            if not skipA:
                phase_A(l)
            if stop == "A":
                break
            if not yT_ext:
                phase_B(l)
            if stop == "B":
                break
            phase_C(l)
            phase_D(l)

        if dbg:
            allr = r_hT + r_hgq + r_hff + r_hiz + r_avz + r_mzz + r_aqT + r_akT + r_mg + sum(r_yT, [])
            srcs = {"hT": hT_t, "hgq": hgq_t, "hff": hff_t, "hiz": hiz_t, "avz": avz_t, "aqT": aqT_t, "mg": mg_t, "yT": yT_t}
            for nm in dbg:
                if nm in srcs:
                    dbg_out(nm, srcs[nm].ap(), allr)
                elif nm.startswith("yT") and len(nm) == 3:
                    dbg_out(nm, yT_t.ap()[int(nm[2])], allr)
        kb.barrier()
    return nc


def host_inputs(inputs):
    f = lambda a: np.ascontiguousarray(np.asarray(a, dtype=np.float32))
    x = f(inputs["x"])
    mem = f(inputs["mem"])
    w_in = f(inputs["w_in"])
    perm = np.arange(INW)
    aq0 = 3072
    hp = np.concatenate([np.arange(0, 64), np.arange(128, 192), np.arange(64, 128), np.arange(192, 256)])
    perm[aq0:aq0 + 256] = aq0 + hp
    w_in_p = np.ascontiguousarray(w_in[:, :, perm])
    pv = np.zeros((L, 128, NPV), np.float32)

    def pk(v, n):
        return np.asarray(v, np.float32).reshape(n, 128).T

    for l in range(L):
        pv[l, :, 0:8] = pk(inputs["norm_pre"][l], 8)
        pv[l, :, 8:16] = pk(inputs["norm_post"][l], 8)
        pv[l, :, 16:24] = pk(inputs["mem_norm"][l], 8)
        dw = np.asarray(inputs["conf_dw_w"][l], np.float32)
        for fc in range(2):
            pv[l, :, 24 + fc * 31:24 + (fc + 1) * 31] = dw[:, fc * 128:(fc + 1) * 128].T
        pv[l, :, 86:88] = pk(inputs["conf_dw_b"][l], 2)
        pv[l, :, 88:90] = pk(inputs["conf_ln_g"][l], 2)
        pv[l, :, 90:92] = pk(inputs["conf_ln_b"][l], 2)
        scw = np.asarray(inputs["sc_w"][l], np.float32)
        for fc in range(2):
            pv[l, :, 92 + fc * 3:92 + (fc + 1) * 3] = scw[:, fc * 128:(fc + 1) * 128].T
    lbl = np.zeros((128, 16), np.float32)
    lg = np.asarray(inputs["hg_lb_logits"], np.float32)
    for l in range(L):
        for d in range(2):
            for fc in range(2):
                lbl[:, l * 4 + d * 2 + fc] = lg[l, d, fc * 128:(fc + 1) * 128]
    bro = np.concatenate([np.asarray(inputs["hg_onorm"], np.float32), np.asarray(inputs["attn_sink"], np.float32)], axis=1)
    c = host_consts()
    shared = {
        "w_in": w_in_p, "w_gate": f(inputs["w_gate"]), "w_branch": f(inputs["w_branch"]),
        "w_out": f(inputs["w_out"]), "w_mkv": f(inputs["w_mem_kv"]), "pv": pv, "lbl": lbl,
        "bro": np.ascontiguousarray(bro), "relb": f(inputs["rel_bias"]).reshape(1, 128),
        "ident": c["ident"], "tri": c["tri"], "blockmask": c["blockmask"], "bmask": c["bmask"], "vmask": c["vmask"],
    }
    per = []
    for b in range(x.shape[0]):
        per.append({"xT": np.ascontiguousarray(x[b].T), "memT": np.ascontiguousarray(mem[b].T)})
    return shared, per


def kernel(**inputs):
    shared, per = host_inputs(inputs)
    nc = build()
    in_maps = [dict(shared, **p) for p in per]
    res = run_bass_kernel_spmd(nc, in_maps, core_ids=list(range(8)))
    outs = [np.asarray(r["out"]).T for r in res.results]
    return np.ascontiguousarray(np.stack(outs, axis=0).astype(np.float32))
```

```python
import contextlib
import numpy as np
import ml_dtypes
import concourse.bass as bass
import concourse.mybir as mybir
from concourse.bass_utils import run_bass_kernel_spmd

F32 = mybir.dt.float32
BF16 = mybir.dt.bfloat16
ALU = mybir.AluOpType
AF = mybir.ActivationFunctionType
AX = mybir.AxisListType

D = 1024
S = 4096
L = 4
W = 256
NTB = 8
TB = 512
INW = 4352
EPS = 1e-6
NPV = 98


class Res:
    __slots__ = ("name", "w", "r", "dkey")

    def __init__(self, name, dkey=None):
        self.name = name
        self.w = None
        self.r = {}
        self.dkey = dkey


class KB:
    def __init__(self, nc, es):
        self.nc = nc
        self.es = es
        self.eng = {"pe": nc.tensor, "dve": nc.vector, "act": nc.scalar, "pool": nc.gpsimd,
                    "sync": nc.sync}
        self.semh = {}
        self.cnt = {}
        for e in ("pe", "dve", "act", "pool"):
            self.semh[e] = es.enter_context(nc.semaphore("s_" + e))
            self.cnt[e] = 0
        self.seen = {e: {} for e in self.eng}
        self.ndk = 0
        self.occ = {}
        self.dcache = {}
        self.dkeys = set()

    def new_epoch(self):
        self.occ = {}

    def dkey(self, name):
        i = self.occ.get(name, 0)
        self.occ[name] = i + 1
        ck = (name, i)
        if ck in self.dcache:
            return self.dcache[ck]
        k = "d%d_%s" % (self.ndk, name)
        self.ndk += 1
        self.semh[k] = self.es.enter_context(self.nc.semaphore(k))
        self.cnt[k] = 0
        self.dkeys.add(k)
        self.dcache[ck] = k
        return k

    def res(self, name, dkey=None):
        return Res(name, dkey)

    def _need(self, need, ev):
        if ev is None:
            return
        k, v = ev
        if need.get(k, 0) < v:
            need[k] = v

    def _emit_waits(self, eng, need):
        for k, v in need.items():
            if k in self.dkeys:
                v = self.cnt[k]
            if self.seen[eng].get(k, 0) >= v:
                continue
            self.eng[eng].wait_ge(self.semh[k], v)
            self.seen[eng][k] = v

    def _deps(self, eng, reads, writes, acc):
        need = {}
        for r in reads:
            self._need(need, r.w)
        for w in writes:
            if not (acc and w.w is not None and w.w[0] == acc):
                self._need(need, w.w)
            for ev in w.r.values():
                self._need(need, ev)
        return need

    def op(self, eng, fn, reads=(), writes=(), acc=None):
        need = self._deps(eng, reads, writes, acc)
        self._emit_waits(eng, need)
        ins = fn(self.eng[eng])
        self.cnt[eng] += 1
        ins.then_inc(self.semh[eng], 1)
        ev = (eng, self.cnt[eng])
        for r in reads:
            r.r[eng] = ev
        for w in writes:
            w.w = ev
            w.r = {}
        return ins

    def dma(self, q, out, in_, reads, writes, nowaw=False):
        need = self._deps(q, reads, writes, None)
        if nowaw:
            for w in writes:
                if w.w is not None and w.w[0] == w.dkey and w.w[0] in need:
                    pass
        k = writes[0].dkey
        if self.cnt[k] > 0:
            need[k] = self.cnt[k]
        self._emit_waits(q, need)
        ins = self.eng[q].dma_start(out=out, in_=in_)
        self.cnt[k] += 16
        ins.then_inc(self.semh[k], 16)
        ev = (k, self.cnt[k])
        for r in reads:
            r.r[k] = ev
        for w in writes:
            w.w = ev
            w.r = {}
        return ins

    def barrier(self):
        need = {k: v for k, v in self.cnt.items() if v > 0}
        for e in ("pe", "dve", "act", "pool", "sync"):
            self._emit_waits(e, dict(need))


def _t5_bucket_table():
    import math
    rel = np.arange(-255, 256)
    half = 16
    n = -rel
    ret = np.where(n < 0, half, 0)
    n = np.abs(n)
    max_exact = half // 2
    large = max_exact + (np.log(np.maximum(n, 1).astype(np.float32) / max_exact)
                         / math.log(128 / max_exact) * (half - max_exact)).astype(np.int32)
    large = np.minimum(large, half - 1)
    b = ret + np.where(n < max_exact, n, large)
    return np.asarray(rel), np.asarray(b)


def host_consts():
    c = {}
    c["ident"] = np.eye(128, dtype=np.float32).astype(ml_dtypes.bfloat16)
    s = np.arange(64)[:, None]
    t = np.arange(64)[None, :]
    tri = np.stack([(s <= t), (s >= t)], axis=1).astype(np.float32)
    c["tri"] = tri.astype(ml_dtypes.bfloat16)
    bm = np.zeros((128, 128), np.float32)
    bm[:64, :64] = 1
    bm[64:, 64:] = 1
    c["blockmask"] = bm
    rel, b = _t5_bucket_table()
    sl = np.arange(128)[:, None, None]
    jj = np.arange(3)[None, :, None]
    qq = np.arange(128)[None, None, :]
    relm = (jj - 1) * 128 + sl - qq
    valid = np.abs(relm) <= 128
    bk = b[relm + 255]
    bm_ = np.zeros((128, 32, 3, 128), np.float32)
    for bb in range(32):
        bm_[:, bb] = (valid & (bk == bb))
    c["bmask"] = bm_.reshape(128, 32 * 384).astype(ml_dtypes.bfloat16)
    c["vmask"] = valid.astype(np.float32).reshape(128, 384)
    return c


def build(dbg=None, nlayers=L, stop=None, yT_ext=False, skipA=False, mixers=("sc", "conf", "mem", "attn", "hg"), hg_stop=None):
    nc = bass.Bass("TRN2", target_bir_lowering=False)

    def dram(name, shape, dt, kind="Internal"):
        return nc.dram_tensor(name, list(shape), dt, kind=kind)

    xT_t = dram("xT", [D, S], F32, "ExternalInput")
    memT_t = dram("memT", [D, 256], F32, "ExternalInput")
    win_t = dram("w_in", [L, D, INW], F32, "ExternalInput")
    wg_t = dram("w_gate", [L, 5, D, D], F32, "ExternalInput")
    wb_t = dram("w_branch", [L, 5, W, D], F32, "ExternalInput")
    wo_t = dram("w_out", [L, D, D], F32, "ExternalInput")
    wm_t = dram("w_mkv", [L, D, 512], F32, "ExternalInput")
    pv_t = dram("pv", [L, 128, NPV], F32, "ExternalInput")
    lbl_t = dram("lbl", [128, 16], F32, "ExternalInput")
    bro_t = dram("bro", [L, 68], F32, "ExternalInput")
    relb_t = dram("relb", [1, 128], F32, "ExternalInput")
    ident_t = dram("ident", [128, 128], BF16, "ExternalInput")
    tri_t = dram("tri", [64, 2, 64], BF16, "ExternalInput")
    bmask_t = dram("blockmask", [128, 128], F32, "ExternalInput")
    bkm_t = dram("bmask", [128, 32 * 384], BF16, "ExternalInput")
    vm_t = dram("vmask", [128, 384], F32, "ExternalInput")
    out_t = dram("out", [D, S], F32, "ExternalOutput")

    hT_t = dram("s_hT", [D, S], BF16)
    hgq_t = dram("s_hgq", [W, S], BF16)
    hff_t = dram("s_hff", [2, W, S], F32)
    cab_t = dram("s_cab", [2 * W, S], BF16)
    cz_t = dram("s_cz", [W, S], BF16)
    sbcv_t = dram("s_sbcv", [3 * W, S], BF16)
    sz_t = dram("s_sz", [W, S], BF16)
    aqT_t = dram("s_aqT", [W, S], BF16)
    akT_t = dram("s_akT", [128, S], BF16)
    mqT_t = dram("s_mqT", [W, S], BF16)
    hiz_t = dram("s_hiz", [S, 512], BF16)
    avz_t = dram("s_avz", [S, 384], BF16)
    mzz_t = dram("s_mzz", [S, 256], BF16)
    hqk_t = dram("s_hqk", [2, 2, W, S], BF16)
    yT_t = dram("s_yT", [5, W, S], BF16, "ExternalInput" if yT_ext else "Internal")
    mg_t = dram("s_mgT", [D, S], BF16)
    dbg_ts = {}
    if dbg:
        for nm, (shape, dt) in dbg.items():
            dbg_ts[nm] = dram("dbg_" + nm, shape, dt, "ExternalOutput")

    _sbc = [0]

    def SBT(name, shape, dt):
        _sbc[0] += 1
        return nc.sbuf_tensor("%s_u%d" % (name, _sbc[0]), shape, dt)

    es = contextlib.ExitStack()
    with es:
        kb = KB(nc, es)

        def sbt(name, shape, dt):
            return es.enter_context(SBT(name, list(shape), dt))

        def pst(name, shape, dt):
            return es.enter_context(nc.psum_tensor(name, list(shape), dt))

        def dres(name, n=NTB):
            k = kb.dkey(name)
            return [kb.res("%s%d" % (name, i), k) for i in range(n)]

        r_out = dres("out")
        r_hT = dres("hT")
        r_hgq = dres("hgq")
        r_hff = dres("hff")
        r_cab = dres("cab")
        r_cz = dres("cz")
        r_sbcv = dres("sbcv")
        r_sz = dres("sz")
        r_aqT = dres("aqT")
        r_akT = dres("akT")
        r_mqT = dres("mqT")
        r_hiz = dres("hiz")
        r_avz = dres("avz")
        r_mzz = dres("mzz")
        r_yT = [dres("yT%d" % n) for n in range(5)]
        r_mg = dres("mg")
        r_hqk = dres("hqk")
        r_dbg = dres("dbg", 1)[0]

        NPS = 4
        ps = [pst("ps%d" % i, [128, 512], F32) for i in range(NPS)]
        r_ps = [kb.res("ps%d" % i) for i in range(NPS)]
        pss = pst("pss", [128, 512], F32)
        r_pss = kb.res("pss")
        pacc = pst("pacc", [128, 512], F32)
        r_pacc = kb.res("pacc")
        pstb = [pst("pstb%d" % i, [128, 1024], BF16) for i in range(2)]
        r_pstb = [kb.res("pstb%d" % i) for i in range(2)]
        psi = [0]

        def next_ps():
            i = psi[0] % NPS
            psi[0] += 1
            return ps[i], r_ps[i]

        ident = sbt("ident_sb", [128, 128], BF16)
        r_ident = kb.res("ident", kb.dkey("ident"))
        ones = sbt("ones", [128, 128], F32)
        r_ones = kb.res("ones")
        pv = sbt("pv_sb", [128, L, NPV], F32)
        r_pv = kb.res("pv", kb.dkey("pv"))
        lbt = sbt("lbt", [128, 16], F32)
        omlb = sbt("omlb", [128, 16], F32)
        r_lb = kb.res("lb", kb.dkey("lb"))
        epsc = sbt("epsc", [128, 1], F32)
        r_eps = kb.res("eps")

        kb.dma("sync", ident[:], ident_t.ap(), [], [r_ident])
        kb.dma("sync", pv[:], pv_t.ap().rearrange("l p n -> p l n"), [], [r_pv])
        kb.op("dve", lambda e: e.memset(ones[:], 1.0), [], [r_ones])
        kb.op("dve", lambda e: e.memset(epsc[:], EPS), [], [r_eps])

        with contextlib.ExitStack() as es0:
            lbl = es0.enter_context(SBT("lbl_sb", [128, 16], F32))
            lsum = es0.enter_context(SBT("lsum", [128, 4], F32))
            kb.dma("sync", lbl[:], lbl_t.ap(), [], [r_lb])
            kb.op("act", lambda e: e.activation(out=lbl[:], in_=lbl[:], func=AF.Exp), [r_lb], [r_lb])
            lv = lbl[:].rearrange("p (l c) -> p l c", l=4)
            kb.op("dve", lambda e: e.tensor_tensor(out=lsum[:], in0=lv[:, 0, :], in1=lv[:, 1, :], op=ALU.add), [r_lb], [r_lb])
            kb.op("dve", lambda e: e.tensor_tensor(out=lsum[:], in0=lsum[:], in1=lv[:, 2, :], op=ALU.add), [r_lb], [r_lb])
            kb.op("dve", lambda e: e.tensor_tensor(out=lsum[:], in0=lsum[:], in1=lv[:, 3, :], op=ALU.add), [r_lb], [r_lb])
            kb.op("dve", lambda e: e.reciprocal(out=lsum[:], in_=lsum[:]), [r_lb], [r_lb])
            kb.op("dve", lambda e: e.tensor_tensor(out=lv, in0=lv, in1=lsum[:].unsqueeze(1).broadcast_to([128, 4, 4]), op=ALU.mult), [r_lb], [r_lb])
            lbv = lbt[:].rearrange("p (l c) -> p l c", l=4)
            kb.op("dve", lambda e: e.memset(lbv[:, 0, :], 0.0), [r_lb], [r_lb])
            for l in range(1, 4):
                kb.op("dve", lambda e, l=l: e.tensor_tensor(out=lbv[:, l, :], in0=lbv[:, l - 1, :], in1=lv[:, l, :], op=ALU.add), [r_lb], [r_lb])
            kb.op("dve", lambda e: e.tensor_scalar(out=omlb[:], in0=lbt[:], scalar1=-1.0, scalar2=1.0, op0=ALU.mult, op1=ALU.add), [r_lb], [r_lb])
            kb.barrier()

        memn = sbt("memn", [128, 8, 256], F32)
        r_memn = kb.res("memn", kb.dkey("memn"))
        expb = sbt("expb", [128, 4, 3, 128], BF16)
        r_expb = kb.res("expb")
        with contextlib.ExitStack() as es0:
            msq = es0.enter_context(SBT("msq", [128, 8, 256], F32))
            r_msq = kb.res("msq")
            mrs = es0.enter_context(SBT("mrs", [128, 256], F32))
            r_mrs = kb.res("mrs")
            kb.dma("sync", memn[:], memT_t.ap().rearrange("(k p) t -> p k t", p=128), [], [r_memn])
            kb.op("act", lambda e: e.activation(out=msq[:], in_=memn[:], func=AF.Square), [r_memn], [r_msq])
            for k in range(8):
                kb.op("pe", lambda e, k=k: e.matmul(pss[:, 0:256], lhsT=ones[:], rhs=msq[:, k, :], start=(k == 0), stop=(k == 7)), [r_ones, r_msq], [r_pss], acc="pe")
            kb.op("act", lambda e: e.activation(out=mrs[:], in_=pss[:, 0:256], func=AF.Sqrt, bias=epsc[:], scale=1.0 / D), [r_pss, r_eps], [r_mrs])
            kb.op("dve", lambda e: e.reciprocal(out=mrs[:], in_=mrs[:]), [r_mrs], [r_mrs])
            kb.op("dve", lambda e: e.tensor_tensor(out=memn[:], in0=memn[:], in1=mrs[:].unsqueeze(1).broadcast_to([128, 8, 256]), op=ALU.mult), [r_memn, r_mrs], [r_memn])
            rrow = es0.enter_context(SBT("rrow", [1, 128], F32))
            r_rrow = kb.res("rrow", kb.dkey("rrow"))
            rbc = es0.enter_context(SBT("rbc", [128, 128], F32))
            r_rbc = kb.res("rbc")
            bkm = es0.enter_context(SBT("bkm", [128, 32, 384], BF16))
            r_bkm = kb.res("bkm", kb.dkey("bkm"))
            vmk = es0.enter_context(SBT("vmk", [128, 384], F32))
            r_vmk = kb.res("vmk", kb.dkey("vmk"))
            bacc = es0.enter_context(SBT("bacc", [128, 4, 384], F32))
            r_bacc = kb.res("bacc")
            kb.dma("sync", rrow[:], relb_t.ap(), [], [r_rrow])
            kb.dma("sync", bkm[:], bkm_t.ap().rearrange("p (b n) -> p b n", b=32), [], [r_bkm])
            kb.dma("sync", vmk[:], vm_t.ap(), [], [r_vmk])
            kb.op("pe", lambda e: e.matmul(ps[0][:, 0:128], lhsT=ones[0:1, :], rhs=rrow[:], start=True, stop=True), [r_ones, r_rrow], [r_ps[0]])
            kb.op("dve", lambda e: e.tensor_copy(out=rbc[:], in_=ps[0][:, 0:128]), [r_ps[0]], [r_rbc])
            kb.op("dve", lambda e: e.memset(bacc[:], 0.0), [], [r_bacc])
            for h in range(4):
                for bb in range(32):
                    kb.op("dve", lambda e, h=h, bb=bb: e.scalar_tensor_tensor(out=bacc[:, h, :], in0=bkm[:, bb, :], scalar=rbc[:, bb * 4 + h:bb * 4 + h + 1], in1=bacc[:, h, :], op0=ALU.mult, op1=ALU.add),
                          [r_bkm, r_rbc, r_bacc], [r_bacc])
            kb.op("act", lambda e: e.activation(out=bacc[:], in_=bacc[:], func=AF.Exp), [r_bacc], [r_bacc])
            kb.op("dve", lambda e: e.tensor_tensor(out=expb[:].rearrange("p h j q -> p h (j q)"), in0=bacc[:], in1=vmk[:].unsqueeze(1).broadcast_to([128, 4, 384]), op=ALU.mult), [r_bacc, r_vmk], [r_expb])
            if dbg and "expb" in dbg:
                kb.dma("sync", dbg_ts["expb"].ap(), expb[:].rearrange("p h j q -> p (h j q)"), [r_expb], [r_dbg])
            kb.barrier()

        cast_rr = [0]

        def cast(out_ap, in_ap, reads, writes):
            e = ("dve", "act", "pool")[cast_rr[0] % 3]
            cast_rr[0] += 1
            if e == "act":
                kb.op("act", lambda en: en.copy(out=out_ap, in_=in_ap), reads, writes)
            else:
                kb.op(e, lambda en: en.tensor_copy(out=out_ap, in_=in_ap), reads, writes)

        evac_rr = [0]

        def evac(out_ap, in_ap, reads, writes, eng=None):
            if eng is None:
                eng = ("act", "dve")[evac_rr[0] % 2]
                evac_rr[0] += 1
            if eng == "act":
                kb.op("act", lambda en: en.copy(out=out_ap, in_=in_ap), reads, writes)
            else:
                kb.op("dve", lambda en: en.tensor_copy(out=out_ap, in_=in_ap), reads, writes)

        def load_w_bf16(esl, name, dst, src_fn, nk, ncols, r_dst, colblk=512):
            stg = [esl.enter_context(SBT("%s_stg%d" % (name, i), [128, nk, colblk], F32)) for i in range(2)]
            r_stg = [kb.res("%s_stg%d" % (name, i), kb.dkey(name + "stg")) for i in range(2)]
            i = 0
            for c0 in range(0, ncols, colblk):
                c1 = min(ncols, c0 + colblk)
                kb.dma("sync", stg[i % 2][:, :, 0:c1 - c0], src_fn(c0, c1), [], [r_stg[i % 2]])
                for k in range(nk):
                    cast(dst[:, k, c0:c1], stg[i % 2][:, k, 0:c1 - c0], [r_stg[i % 2]], [r_dst])
                i += 1

        def rms_rstd(esl, name, src, r_src, nk, ncol, ptile, r_ptile):
            sq = esl.enter_context(SBT(name + "_sq", [128, nk, ncol], F32))
            r_sq = kb.res(name + "_sq")
            rstd = esl.enter_context(SBT(name + "_rstd", [128, ncol], F32))
            r_rstd = kb.res(name + "_rstd")
            return sq, r_sq, rstd, r_rstd

        def phase_A(l):
            kb.new_epoch()
            src_t = xT_t if l == 0 else out_t
            with contextlib.ExitStack() as esl:
                wsb = esl.enter_context(SBT("A_w", [128, 8, INW], BF16))
                r_w = kb.res("A_w")
                with contextlib.ExitStack() as esw:
                    load_w_bf16(esw, "A", wsb, lambda c0, c1: win_t.ap()[l].rearrange("(k p) c -> p k c", p=128)[:, :, c0:c1], 8, INW, r_w)
                    kb.barrier()
                xt = [esl.enter_context(SBT("A_x%d" % i, [128, 8, TB], F32)) for i in range(2)]
                r_xt = [kb.res("A_x%d" % i, kb.dkey("A_x")) for i in range(2)]
                sq = esl.enter_context(SBT("A_sq", [128, 8, TB], F32))
                r_sq = kb.res("A_sq")
                rstd = esl.enter_context(SBT("A_rstd", [128, TB], F32))
                r_rstd = kb.res("A_rstd")
                hT = [esl.enter_context(SBT("A_h%d" % i, [128, 8, TB], BF16)) for i in range(2)]
                r_h = [kb.res("A_h%d" % i) for i in range(2)]
                NO = 3
                ob = [esl.enter_context(SBT("A_ob%d" % i, [128, 6, TB], BF16)) for i in range(NO)]
                r_ob = [kb.res("A_ob%d" % i) for i in range(NO)]
                of = [esl.enter_context(SBT("A_of%d" % i, [128, 2, TB], F32)) for i in range(2)]
                r_of = [kb.res("A_of%d" % i) for i in range(2)]
                oi = [0]
                ofi = [0]
                xv = src_t.ap().rearrange("(k p) t -> p k t", p=128)
                hv = hT_t.ap().rearrange("(k p) t -> p k t", p=128)
                kb.dma("sync", xt[0][:], xv[:, :, 0:TB], [r_out[0]], [r_xt[0]])
                for tb in range(NTB):
                    t0 = tb * TB
                    X = xt[tb % 2]
                    rX = r_xt[tb % 2]
                    if tb + 1 < NTB:
                        kb.dma("sync", xt[(tb + 1) % 2][:], xv[:, :, t0 + TB:t0 + 2 * TB], [r_out[tb + 1]], [r_xt[(tb + 1) % 2]])
                    kb.op("act", lambda e: e.activation(out=sq[:], in_=X[:], func=AF.Square), [rX], [r_sq])
                    for k in range(8):
                        kb.op("pe", lambda e, k=k: e.matmul(pss[:], lhsT=ones[:], rhs=sq[:, k, :], start=(k == 0), stop=(k == 7)),
                              [r_ones, r_sq], [r_pss], acc="pe")
                    kb.op("act", lambda e: e.activation(out=rstd[:], in_=pss[:], func=AF.Sqrt, bias=epsc[:], scale=1.0 / D), [r_pss, r_eps], [r_rstd])
                    kb.op("dve", lambda e: e.reciprocal(out=rstd[:], in_=rstd[:]), [r_rstd], [r_rstd])
                    H = hT[tb % 2]
                    rH = r_h[tb % 2]
                    for k in range(8):
                        kb.op("dve", lambda e, k=k: e.scalar_tensor_tensor(out=H[:, k, :], in0=X[:, k, :], scalar=pv[:, l, k:k + 1], in1=rstd[:], op0=ALU.mult, op1=ALU.mult),
                              [rX, r_pv, r_rstd], [rH])
                    kb.dma("pool", hv[:, :, t0:t0 + TB], H[:], [rH], [r_hT[tb]])

                    def fm_group(col0, nblk, dst_ap_fn, r_dst, f32=False):
                        if f32:
                            o = of[ofi[0] % 2]
                            ro = r_of[ofi[0] % 2]
                            ofi[0] += 1
                        else:
                            o = ob[oi[0] % NO]
                            ro = r_ob[oi[0] % NO]
                            oi[0] += 1
                        for b in range(nblk):
                            p, rp = next_ps()
                            for k in range(8):
                                kb.op("pe", lambda e, k=k, b=b: e.matmul(p[:], lhsT=wsb[:, k, col0 + b * 128: col0 + (b + 1) * 128], rhs=H[:, k, :], start=(k == 0), stop=(k == 7)),
                                      [r_w, rH], [rp], acc="pe")
                            evac(o[:, b, :], p[:], [rp], [ro])
                        kb.dma("pool", dst_ap_fn(t0), o[:, 0:nblk, :], [ro], [r_dst[tb]])

                    def tm_group(col0, ncol, dst_t, r_dst):
                        o = ob[oi[0] % NO]
                        ro = r_ob[oi[0] % NO]
                        oi[0] += 1
                        ov = o[:].rearrange("p a t -> p (a t)")[:, 0:4 * ncol].rearrange("p (s c) -> p s c", s=4)
                        for ts in range(4):
                            p, rp = next_ps()
                            for k in range(8):
                                kb.op("pe", lambda e, k=k, ts=ts: e.matmul(p[:, 0:ncol], lhsT=H[:, k, ts * 128:(ts + 1) * 128], rhs=wsb[:, k, col0:col0 + ncol], start=(k == 0), stop=(k == 7)),
                                      [r_w, rH], [rp], acc="pe")
                            evac(ov[:, ts, :], p[:, 0:ncol], [rp], [ro])
                        kb.dma("pool", dst_t.ap()[t0:t0 + TB, :].rearrange("(s p) c -> p s c", p=128), ov, [ro], [r_dst[tb]])

                    fmv = lambda t, r0, nb: (lambda tt: t.ap()[r0:r0 + nb * 128, :].rearrange("(b p) t -> p b t", p=128)[:, :, tt:tt + TB])
                    fm_group(0, 2, fmv(hgq_t, 0, 2), r_hgq)
                    fm_group(256, 2, lambda tt: hff_t.ap()[0].rearrange("(b p) t -> p b t", p=128)[:, :, tt:tt + TB], r_hff, f32=True)
                    fm_group(512, 2, lambda tt: hff_t.ap()[1].rearrange("(b p) t -> p b t", p=128)[:, :, tt:tt + TB], r_hff, f32=True)
                    tm_group(768, 512, hiz_t, r_hiz)
                    fm_group(1280, 4, fmv(cab_t, 0, 4), r_cab)
                    fm_group(1792, 2, fmv(cz_t, 0, 2), r_cz)
                    fm_group(2048, 6, fmv(sbcv_t, 0, 6), r_sbcv)
                    fm_group(2816, 2, fmv(sz_t, 0, 2), r_sz)
                    fm_group(3072, 2, fmv(aqT_t, 0, 2), r_aqT)
                    fm_group(3328, 1, fmv(akT_t, 0, 1), r_akT)
                    tm_group(3456, 384, avz_t, r_avz)
                    fm_group(3840, 2, fmv(mqT_t, 0, 2), r_mqT)
                    tm_group(4096, 256, mzz_t, r_mzz)
                kb.barrier()

        yTv = [yT_t.ap()[n].rearrange("(k p) t -> p k t", p=128) for n in range(5)]

        def build_diag(dst, fc, k, col):
            kb.op("dve", lambda e: e.tensor_scalar_mul(out=dst[:, fc, k, :], in0=ident[:], scalar1=col), [r_ident, r_pv], [r_dg])

        r_dg = kb.res("dg")

        def B_sc(l):
            kb.new_epoch()
            with contextlib.ExitStack() as esl:
                dg = esl.enter_context(SBT("sc_dg", [128, 2, 3, 128], BF16))
                for fc in range(2):
                    for k in range(3):
                        build_diag(dg, fc, k, pv[:, l, 92 + fc * 3 + k:93 + fc * 3 + k])
                tin = [esl.enter_context(SBT("sc_in%d" % i, [128, 6, TB + 2], BF16)) for i in range(2)]
                r_in = [kb.res("sc_in%d" % i, kb.dkey("sc_in")) for i in range(2)]
                tz = [esl.enter_context(SBT("sc_z%d" % i, [128, 2, TB], BF16)) for i in range(2)]
                r_z = [kb.res("sc_z%d" % i, kb.dkey("sc_z")) for i in range(2)]
                cv = esl.enter_context(SBT("sc_cv", [128, 2, TB + 2], BF16))
                r_cv = kb.res("sc_cv")
                sl = esl.enter_context(SBT("sc_sl", [128, 2, TB], BF16))
                r_sl = kb.res("sc_sl")
                yo = [esl.enter_context(SBT("sc_yo%d" % i, [128, 2, TB], BF16)) for i in range(2)]
                r_yo = [kb.res("sc_yo%d" % i) for i in range(2)]
                inv = sbcv_t.ap().rearrange("(b p) t -> p b t", p=128)
                zv = sz_t.ap().rearrange("(b p) t -> p b t", p=128)

                def loads(tb):
                    t0 = tb * TB
                    T = tin[tb % 2]
                    rT = r_in[tb % 2]
                    lo = max(t0 - 1, 0)
                    hi = min(t0 + TB + 1, S)
                    if tb == 0:
                        kb.op("dve", lambda e: e.memset(T[:, :, 0:1], 0.0), [], [rT])
                    if tb == NTB - 1:
                        kb.op("dve", lambda e: e.memset(T[:, :, TB + 1:TB + 2], 0.0), [], [rT])
                    kb.dma("sync", T[:, :, lo - (t0 - 1):hi - (t0 - 1)], inv[:, :, lo:hi], [r_sbcv[tb], r_sbcv[max(tb - 1, 0)], r_sbcv[min(tb + 1, NTB - 1)]], [rT])
                    kb.dma("sync", tz[tb % 2][:], zv[:, :, t0:t0 + TB], [r_sz[tb]], [r_z[tb % 2]])

                loads(0)
                for tb in range(NTB):
                    t0 = tb * TB
                    if tb + 1 < NTB:
                        loads(tb + 1)
                    T = tin[tb % 2]
                    rT = r_in[tb % 2]
                    kb.op("dve", lambda e: e.tensor_tensor(out=cv[:], in0=T[:, 2:4, :], in1=T[:, 4:6, :], op=ALU.mult), [rT], [r_cv])
                    kb.op("act", lambda e: e.activation(out=sl[:], in_=tz[tb % 2][:], func=AF.Silu), [r_z[tb % 2]], [r_sl])
                    kb.op("dve", lambda e: e.tensor_tensor(out=sl[:], in0=sl[:], in1=T[:, 0:2, 1:TB + 1], op=ALU.mult), [r_sl, rT], [r_sl])
                    Y = yo[tb % 2]
                    rY = r_yo[tb % 2]
                    for fc in range(2):
                        p, rp = next_ps()
                        for k in range(3):
                            kb.op("pe", lambda e, k=k, fc=fc: e.matmul(p[:], lhsT=dg[:, fc, k, :], rhs=cv[:, fc, k:k + TB], start=(k == 0), stop=(k == 2)),
                                  [r_dg, r_cv], [rp], acc="pe")
                        kb.op("dve", lambda e, fc=fc: e.tensor_tensor(out=Y[:, fc, :], in0=p[:], in1=sl[:, fc, :], op=ALU.mult), [rp, r_sl], [rY])
                    kb.dma("pool", yTv[2][:, :, t0:t0 + TB], Y[:], [rY], [r_yT[2][tb]])
                kb.barrier()

        def B_conf(l):
            kb.new_epoch()
            HL = 15
            with contextlib.ExitStack() as esl:
                dg = esl.enter_context(SBT("cf_dg", [128, 2, 31, 128], BF16))
                for fc in range(2):
                    for k in range(31):
                        build_diag(dg, fc, k, pv[:, l, 24 + fc * 31 + k:25 + fc * 31 + k])
                tin = [esl.enter_context(SBT("cf_in%d" % i, [128, 4, TB + 2 * HL], BF16)) for i in range(2)]
                r_in = [kb.res("cf_in%d" % i, kb.dkey("cf_in")) for i in range(2)]
                tz = [esl.enter_context(SBT("cf_z%d" % i, [128, 2, TB], BF16)) for i in range(2)]
                r_z = [kb.res("cf_z%d" % i, kb.dkey("cf_z")) for i in range(2)]
                u = esl.enter_context(SBT("cf_u", [128, 2, TB + 2 * HL], BF16))
                r_u = kb.res("cf_u")
                csb = esl.enter_context(SBT("cf_c", [128, 2, TB], F32))
                r_c = kb.res("cf_c")
                csq = esl.enter_context(SBT("cf_csq", [128, 2, TB], F32))
                r_csq = kb.res("cf_csq")
                mu = esl.enter_context(SBT("cf_mu", [128, TB], F32))
                r_mu = kb.res("cf_mu")
                var = esl.enter_context(SBT("cf_var", [128, TB], F32))
                r_var = kb.res("cf_var")
                sn = esl.enter_context(SBT("cf_sn", [128, 2, TB], BF16))
                r_sn = kb.res("cf_sn")
                sl = esl.enter_context(SBT("cf_sl", [128, 2, TB], BF16))
                r_sl = kb.res("cf_sl")
                yo = [esl.enter_context(SBT("cf_yo%d" % i, [128, 2, TB], BF16)) for i in range(2)]
                r_yo = [kb.res("cf_yo%d" % i) for i in range(2)]
                inv = cab_t.ap().rearrange("(b p) t -> p b t", p=128)
                zv = cz_t.ap().rearrange("(b p) t -> p b t", p=128)

                def loads(tb):
                    t0 = tb * TB
                    T = tin[tb % 2]
                    rT = r_in[tb % 2]
                    lo = max(t0 - HL, 0)
                    hi = min(t0 + TB + HL, S)
                    if tb == 0:
                        kb.op("dve", lambda e: e.memset(T[:, :, 0:HL], 0.0), [], [rT])
                    if tb == NTB - 1:
                        kb.op("dve", lambda e: e.memset(T[:, :, TB + HL:TB + 2 * HL], 0.0), [], [rT])
                    kb.dma("sync", T[:, :, lo - (t0 - HL):hi - (t0 - HL)], inv[:, :, lo:hi], [r_cab[tb], r_cab[max(tb - 1, 0)], r_cab[min(tb + 1, NTB - 1)]], [rT])
                    kb.dma("sync", tz[tb % 2][:], zv[:, :, t0:t0 + TB], [r_cz[tb]], [r_z[tb % 2]])

                loads(0)
                for tb in range(NTB):
                    t0 = tb * TB
                    if tb + 1 < NTB:
                        loads(tb + 1)
                    T = tin[tb % 2]
                    rT = r_in[tb % 2]
                    kb.op("act", lambda e: e.activation(out=u[:], in_=T[:, 2:4, :], func=AF.Sigmoid), [rT], [r_u])
                    kb.op("dve", lambda e: e.tensor_tensor(out=u[:], in0=u[:], in1=T[:, 0:2, :], op=ALU.mult), [r_u, rT], [r_u])
                    for fc in range(2):
                        p, rp = next_ps()
                        for k in range(31):
                            kb.op("pe", lambda e, k=k, fc=fc: e.matmul(p[:], lhsT=dg[:, fc, k, :], rhs=u[:, fc, k:k + TB], start=(k == 0), stop=(k == 30)),
                                  [r_dg, r_u], [rp], acc="pe")
                        kb.op("act", lambda e, fc=fc: e.activation(out=csb[:, fc, :], in_=p[:], func=AF.Identity, bias=pv[:, l, 86 + fc:87 + fc], scale=1.0), [rp, r_pv], [r_c])
                    kb.op("act", lambda e: e.activation(out=csq[:], in_=csb[:], func=AF.Square), [r_c], [r_csq])
                    for fc in range(2):
                        kb.op("pe", lambda e, fc=fc: e.matmul(pss[:], lhsT=ones[:], rhs=csb[:, fc, :], start=(fc == 0), stop=(fc == 1)), [r_ones, r_c], [r_pss], acc="pe")
                    for fc in range(2):
                        kb.op("pe", lambda e, fc=fc: e.matmul(pacc[:], lhsT=ones[:], rhs=csq[:, fc, :], start=(fc == 0), stop=(fc == 1)), [r_ones, r_csq], [r_pacc], acc="pe")
                    kb.op("dve", lambda e: e.tensor_scalar_mul(out=mu[:], in0=pss[:], scalar1=1.0 / W), [r_pss], [r_mu])
                    kb.op("dve", lambda e: e.tensor_tensor(out=var[:], in0=mu[:], in1=mu[:], op=ALU.mult), [r_mu], [r_var])
                    kb.op("dve", lambda e: e.scalar_tensor_tensor(out=var[:], in0=pacc[:], scalar=1.0 / W, in1=var[:], op0=ALU.mult, op1=ALU.subtract), [r_pacc, r_var], [r_var])
                    kb.op("act", lambda e: e.activation(out=var[:], in_=var[:], func=AF.Sqrt, bias=epsc[:], scale=1.0), [r_var, r_eps], [r_var])
                    kb.op("dve", lambda e: e.reciprocal(out=var[:], in_=var[:]), [r_var], [r_var])
                    Y = yo[tb % 2]
                    rY = r_yo[tb % 2]
                    kb.op("act", lambda e: e.activation(out=sl[:], in_=tz[tb % 2][:], func=AF.Silu), [r_z[tb % 2]], [r_sl])
                    for fc in range(2):
                        kb.op("dve", lambda e, fc=fc: e.tensor_tensor(out=csb[:, fc, :], in0=csb[:, fc, :], in1=mu[:], op=ALU.subtract), [r_c, r_mu], [r_c])
                        kb.op("dve", lambda e, fc=fc: e.tensor_tensor(out=csb[:, fc, :], in0=csb[:, fc, :], in1=var[:], op=ALU.mult), [r_c, r_var], [r_c])
                        kb.op("act", lambda e, fc=fc: e.activation(out=sn[:, fc, :], in_=csb[:, fc, :], func=AF.Silu, bias=pv[:, l, 90 + fc:91 + fc], scale=pv[:, l, 88 + fc:89 + fc]), [r_c, r_pv], [r_sn])
                    kb.op("dve", lambda e: e.tensor_tensor(out=Y[:], in0=sn[:], in1=sl[:], op=ALU.mult), [r_sn, r_sl], [rY])
                    kb.dma("pool", yTv[1][:, :, t0:t0 + TB], Y[:], [rY], [r_yT[1][tb]])
                kb.barrier()

        def finish_tm(esl, name):
            ya = esl.enter_context(SBT(name + "_ya", [128, 4, 64], F32))
            sl = esl.enter_context(SBT(name + "_sl", [128, 256], F32))
            ym = [esl.enter_context(SBT(name + "_ym%d" % i, [128, 256], BF16)) for i in range(2)]
            yo = [esl.enter_context(SBT(name + "_yo%d" % i, [128, 2, TB], BF16)) for i in range(2)]
            rd = esl.enter_context(SBT(name + "_rd", [128, 4], F32))
            return (ya, kb.res(name + "_ya"), sl, kb.res(name + "_sl"), ym, [kb.res(name + "_ym%d" % i) for i in range(2)],
                    yo, [kb.res(name + "_yo%d" % i) for i in range(2)], rd, kb.res(name + "_rd"))

        def tm_epilogue(F, idx, o_tile, r_o, z_ap, r_z, esink, r_es, q128, bi):
            ya, r_ya, sl, r_sl, ym, r_ym, yo, r_yo, rd, r_rd = F
            ov = o_tile[:, 0:260].rearrange("p (h e) -> p h e", e=65)
            if esink is not None:
                kb.op("dve", lambda e: e.tensor_tensor(out=rd[:], in0=ov[:, :, 64], in1=esink[:], op=ALU.add), [r_o, r_es], [r_rd])
            else:
                kb.op("dve", lambda e: e.tensor_copy(out=rd[:], in_=ov[:, :, 64]), [r_o], [r_rd])
            kb.op("dve", lambda e: e.reciprocal(out=rd[:], in_=rd[:]), [r_rd], [r_rd])
            kb.op("dve", lambda e: e.tensor_tensor(out=ya[:], in0=ov[:, :, 0:64], in1=rd[:].unsqueeze(2).broadcast_to([128, 4, 64]), op=ALU.mult), [r_o, r_rd], [r_ya])
            kb.op("act", lambda e: e.activation(out=sl[:], in_=z_ap, func=AF.Silu), [r_z], [r_sl])
            m = idx % 2
            kb.op("dve", lambda e: e.tensor_tensor(out=ym[m][:], in0=ya[:].rearrange("p h e -> p (h e)"), in1=sl[:], op=ALU.mult), [r_ya, r_sl], [r_ym[m]])
            if dbg and "eYA" in dbg and bi == 3 and q128 == 5:
                kb.dma("sync", dbg_ts["eYA"].ap(), ya[:].rearrange("p h e -> p (h e)"), [r_ya], [r_dbg])
                kb.dma("sync", dbg_ts["eSL"].ap(), sl[:], [r_sl], [r_dbg])
                kb.dma("sync", dbg_ts["eYM"].ap(), ym[m][:], [r_ym[m]], [r_dbg])
                kb.dma("sync", dbg_ts["eRD"].ap(), rd[:], [r_rd], [r_dbg])
            sub = q128 % 4
            for half in range(2):
                kb.op("pe", lambda e, half=half: e.transpose(out=pstb[half][:, sub * 128:(sub + 1) * 128], in_=ym[m][:, half * 128:(half + 1) * 128], identity=ident[:]),
                      [r_ym[m], r_ident], [r_pstb[half]], acc="pe")
            if sub == 3:
                tbi = q128 // 4
                Y = yo[tbi % 2]
                rY = r_yo[tbi % 2]
                for half in range(2):
                    evac(Y[:, half, :], pstb[half][:, 0:TB], [r_pstb[half]], [rY])
                if dbg and "eY" in dbg and bi == 3 and tbi == 1:
                    kb.dma("sync", dbg_ts["eY"].ap(), Y[:].rearrange("p k t -> p (k t)"), [rY], [r_dbg])
                kb.dma("pool", yTv[bi][:, :, tbi * TB:(tbi + 1) * TB], Y[:], [rY], [r_yT[bi][tbi]])

        def B_mem(l):
            kb.new_epoch()
            with contextlib.ExitStack() as esl:
                wm = esl.enter_context(SBT("m_w", [128, 8, 512], BF16))
                r_wm = kb.res("m_w")
                with contextlib.ExitStack() as esw:
                    load_w_bf16(esw, "M", wm, lambda c0, c1: wm_t.ap()[l].rearrange("(k p) c -> p k c", p=128)[:, :, c0:c1], 8, 512, r_wm)
                    kb.barrier()
                memh = esl.enter_context(SBT("m_h", [128, 8, 256], BF16))
                r_memh = kb.res("m_h")
                for k in range(8):
                    kb.op("dve", lambda e, k=k: e.tensor_scalar_mul(out=memh[:, k, :], in0=memn[:, k, :], scalar1=pv[:, l, 16 + k:17 + k]), [r_memn, r_pv], [r_memh])
                kT = esl.enter_context(SBT("m_kT", [128, 2, 256], BF16))
                r_kT = kb.res("m_kT")
                vaug = esl.enter_context(SBT("m_va", [128, 2, 4, 65], BF16))
                r_va = kb.res("m_va")
                kb.op("dve", lambda e: e.memset(vaug[:], 1.0), [], [r_va])
                for pair in range(2):
                    p, rp = next_ps()
                    for k in range(8):
                        kb.op("pe", lambda e, k=k, pair=pair: e.matmul(p[:, 0:256], lhsT=wm[:, k, pair * 128:(pair + 1) * 128], rhs=memh[:, k, :], start=(k == 0), stop=(k == 7)), [r_wm, r_memh], [rp], acc="pe")
                    evac(kT[:, pair, :], p[:, 0:256], [rp], [r_kT])
                for mb in range(2):
                    p, rp = next_ps()
                    for k in range(8):
                        kb.op("pe", lambda e, k=k, mb=mb: e.matmul(p[:, 0:256], lhsT=memh[:, k, mb * 128:(mb + 1) * 128], rhs=wm[:, k, 256:512], start=(k == 0), stop=(k == 7)), [r_wm, r_memh], [rp], acc="pe")
                    evac(vaug[:, mb, :, 0:64], p[:, 0:256].rearrange("p (h e) -> p h e", e=64), [rp], [r_va])
                tq = [esl.enter_context(SBT("m_q%d" % i, [128, 2, TB], BF16)) for i in range(2)]
                r_q = [kb.res("m_q%d" % i, kb.dkey("m_q")) for i in range(2)]
                tz = [esl.enter_context(SBT("m_z%d" % i, [128, 4, 256], BF16)) for i in range(2)]
                r_z = [kb.res("m_z%d" % i, kb.dkey("m_z")) for i in range(2)]
                pT = [esl.enter_context(SBT("m_pT%d" % i, [128, 2, TB], BF16)) for i in range(2)]
                r_pT = [kb.res("m_pT%d" % i) for i in range(2)]
                F = finish_tm(esl, "m")
                qv = mqT_t.ap().rearrange("(b p) t -> p b t", p=128)
                otl = [pss, pacc, ps[2], ps[3]]
                r_otl = [r_pss, r_pacc, r_ps[2], r_ps[3]]

                def loads(tb):
                    t0 = tb * TB
                    kb.dma("sync", tq[tb % 2][:], qv[:, :, t0:t0 + TB], [r_mqT[tb]], [r_q[tb % 2]])
                    kb.dma("sync", tz[tb % 2][:], mzz_t.ap()[t0:t0 + TB, :].rearrange("(s p) c -> p s c", p=128), [r_mzz[tb]], [r_z[tb % 2]])

                loads(0)
                ci = 0
                for tb in range(NTB):
                    if tb + 1 < NTB:
                        loads(tb + 1)
                    Q = tq[tb % 2]
                    rQ = r_q[tb % 2]
                    for h in range(4):
                        pair, base = h // 2, (h % 2) * 64
                        P = pT[ci % 2]
                        rP = r_pT[ci % 2]
                        ci += 1
                        for mb in range(2):
                            sc_, rsc = ps[mb], r_ps[mb]
                            kb.op("pe", lambda e, mb=mb: e.matmul(sc_[:], lhsT=kT[base:base + 64, pair, mb * 128:(mb + 1) * 128], rhs=Q[base:base + 64, pair, :], start=True, stop=True), [r_kT, rQ], [rsc])
                            kb.op("act", lambda e, mb=mb: e.activation(out=P[:, mb, :], in_=sc_[:], func=AF.Exp, scale=0.125), [rsc], [rP])
                        for ts in range(4):
                            ov = otl[ts][:, 0:260].rearrange("p (h e) -> p h e", e=65)
                            for mb in range(2):
                                kb.op("pe", lambda e, mb=mb, ts=ts: e.matmul(ov[:, h, :], lhsT=P[:, mb, ts * 128:(ts + 1) * 128], rhs=vaug[:, mb, h, :], start=(mb == 0), stop=(mb == 1)),
                                      [rP, r_va], [r_otl[ts]], acc=("pe" if (mb > 0 or h > 0) else None))
                    for ts in range(4):
                        tm_epilogue(F, tb * 4 + ts, otl[ts], r_otl[ts], tz[tb % 2][:, ts, :], r_z[tb % 2], None, None, tb * 4 + ts, 4)
                kb.barrier()

        def B_attn(l):
            kb.new_epoch()
            NB = 32
            with contextlib.ExitStack() as esl:
                akT = esl.enter_context(SBT("a_k", [128, S], BF16))
                r_ak = kb.res("a_k", kb.dkey("a_k"))
                aqT = esl.enter_context(SBT("a_q", [128, 2, S], BF16))
                r_aq = kb.res("a_q", kb.dkey("a_q"))
                vz = esl.enter_context(SBT("a_vz", [128, NB, 384], BF16))
                r_vz = kb.res("a_vz", kb.dkey("a_vz"))
                vaug = esl.enter_context(SBT("a_va", [128, NB, 2, 66], BF16))
                r_va = kb.res("a_va")
                esk = esl.enter_context(SBT("a_es", [128, 4], F32))
                r_es = kb.res("a_es")
                kb.dma("sync", akT[:], akT_t.ap(), r_akT, [r_ak])
                kb.dma("sync", aqT[:], aqT_t.ap().rearrange("(b p) t -> p b t", p=128), r_aqT, [r_aq])
                kb.dma("sync", vz[:], avz_t.ap().rearrange("(b p) c -> p b c", p=128), r_avz, [r_vz])
                srow = esl.enter_context(SBT("a_srow", [1, 4], F32))
                r_srow = kb.res("a_srow", kb.dkey("a_srow"))
                kb.dma("sync", srow[:], bro_t.ap()[l:l + 1, 64:68], [], [r_srow])
                kb.op("pe", lambda e: e.matmul(ps[0][:, 0:4], lhsT=ones[0:1, :], rhs=srow[:], start=True, stop=True), [r_ones, r_srow], [r_ps[0]])
                kb.op("act", lambda e: e.activation(out=esk[:], in_=ps[0][:, 0:4], func=AF.Exp), [r_ps[0]], [r_es])
                kb.op("dve", lambda e: e.memset(vaug[:], 1.0), [], [r_va])
                kb.op("dve", lambda e: e.tensor_copy(out=vaug[:, :, :, 0:64], in_=vz[:, :, 0:128].rearrange("p b (k d) -> p b k d", d=64)), [r_vz], [r_va])
                et = [esl.enter_context(SBT("a_e%d" % i, [128, 3, 128], BF16)) for i in range(2)]
                r_et = [kb.res("a_e%d" % i) for i in range(2)]
                pT = [esl.enter_context(SBT("a_p%d" % i, [128, 3, 128], BF16)) for i in range(2)]
                r_pT = [kb.res("a_p%d" % i) for i in range(2)]
                F = finish_tm(esl, "a")
                otl = [pss, pacc, ps[2], ps[3]]
                r_otl = [r_pss, r_pacc, r_ps[2], r_ps[3]]
                ci = 0
                for n in range(NB):
                    O = otl[n % 4]
                    rO = r_otl[n % 4]
                    ov = O[:, 0:260].rearrange("p (h e) -> p h e", e=65)
                    js = [j for j in range(3) if 0 <= n - 1 + j < NB]
                    def a_scores(h):
                        chunk, base = h % 2, (h // 2) * 64
                        i2 = (n * 4 + h) % 2
                        sc_, rsc = ps[i2], r_ps[i2]
                        E, rE = et[i2], r_et[i2]
                        P, rP = pT[i2], r_pT[i2]
                        for j in js:
                            kb.op("pe", lambda e, j=j: e.matmul(sc_[:, j * 128:(j + 1) * 128], lhsT=akT[base:base + 64, (n - 1 + j) * 128:(n + j) * 128], rhs=aqT[base:base + 64, chunk, n * 128:(n + 1) * 128], start=True, stop=True),
                                  [r_ak, r_aq], [rsc], acc="pe")
                        j0, j1 = js[0], js[-1] + 1
                        kb.op("act", lambda e: e.activation(out=E[:, j0:j1, :], in_=sc_[:, j0 * 128:j1 * 128].rearrange("p (j q) -> p j q", q=128), func=AF.Exp, scale=0.125), [rsc], [rE])
                        kb.op("dve", lambda e: e.tensor_tensor(out=P[:, j0:j1, :], in0=E[:, j0:j1, :], in1=expb[:, h, j0:j1, :], op=ALU.mult), [rE, r_expb], [rP])

                    a_scores(0)
                    for h in range(4):
                        kvh = h // 2
                        i2 = (n * 4 + h) % 2
                        P, rP = pT[i2], r_pT[i2]
                        if h + 1 < 4:
                            a_scores(h + 1)
                        for j in js:
                            kb.op("pe", lambda e, j=j: e.matmul(ov[:, h, :], lhsT=P[:, j, :], rhs=vaug[:, n - 1 + j, kvh, 0:65], start=(j == js[0]), stop=(j == js[-1])),
                                  [rP, r_va], [rO], acc=("pe" if (j != js[0] or h > 0) else None))
                    tm_epilogue(F, n, O, rO, vz[:, n, 128:384], r_vz, esk, r_es, n, 3)
                kb.barrier()

        def B_hg(l):
            kb.new_epoch()
            GT = 1024
            NG1 = S // GT
            CG = GT // 64
            with contextlib.ExitStack() as esl:
                ev = esl.enter_context(SBT("h_ev", [128, 3, 2, 2, 64], F32))
                r_ev = kb.res("h_ev")
                with contextlib.ExitStack() as es1:
                    rmask = es1.enter_context(SBT("h_rm", [128, GT], BF16))
                    r_rm = kb.res("h_rm")
                    kb.op("dve", lambda e: e.memset(rmask[:], 1.0), [], [r_rm])
                    kb.op("dve", lambda e: e.memset(rmask[:].rearrange("p (c t) -> p c t", t=64)[:, :, 0:1], 0.0), [], [r_rm])
                    tq = [es1.enter_context(SBT("h_q%d" % i, [128, 2, GT], BF16)) for i in range(2)]
                    r_tq = [kb.res("h_q%d" % i, kb.dkey("h_q")) for i in range(2)]
                    tl = [es1.enter_context(SBT("h_l%d" % i, [128, GT], F32)) for i in range(2)]
                    r_tl = [kb.res("h_l%d" % i, kb.dkey("h_l")) for i in range(2)]
                    tg = es1.enter_context(SBT("h_g", [128, GT], F32))
                    r_tg = kb.res("h_g")
                    ta = es1.enter_context(SBT("h_a", [128, GT], F32))
                    r_ta = kb.res("h_a")
                    tb_ = es1.enter_context(SBT("h_b", [128, GT], F32))
                    r_tb = kb.res("h_b")
                    tc_ = es1.enter_context(SBT("h_c", [128, GT], F32))
                    r_tc = kb.res("h_c")
                    te = es1.enter_context(SBT("h_e", [128, GT], F32))
                    r_te = kb.res("h_e")
                    t16 = es1.enter_context(SBT("h_16", [128, CG], F32))
                    r_t16 = kb.res("h_16")
                    oq = [es1.enter_context(SBT("h_oq%d" % i, [128, GT], BF16)) for i in range(2)]
                    r_oq = [kb.res("h_oq%d" % i) for i in range(2)]
                    ok = [es1.enter_context(SBT("h_ok%d" % i, [128, GT], BF16)) for i in range(2)]
                    r_ok = [kb.res("h_ok%d" % i) for i in range(2)]
                    qv = hgq_t.ap().rearrange("(b p) t -> p b t", p=128)
                    it = 0
                    for g in range(NG1):
                        g0 = g * GT
                        rsrc = [r_hgq[2 * g], r_hgq[2 * g + 1]]
                        Q = tq[g % 2]
                        rQ = r_tq[g % 2]
                        kb.dma("sync", Q[:], qv[:, :, g0:g0 + GT], rsrc, [rQ])
                        for d in range(2):
                            for fc in range(2):
                                col = l * 4 + d * 2 + fc
                                TL = tl[it % 2]
                                rTL = r_tl[it % 2]
                                OQ, rOQ, OK, rOK = oq[it % 2], r_oq[it % 2], ok[it % 2], r_ok[it % 2]
                                it += 1
                                kb.dma("sync", TL[:], hff_t.ap()[d, fc * 128:(fc + 1) * 128, g0:g0 + GT], [r_hff[2 * g], r_hff[2 * g + 1]], [rTL])
                                kb.op("act", lambda e: e.activation(out=TL[:], in_=TL[:], func=AF.Sigmoid), [rTL], [rTL])
                                kb.op("dve", lambda e: e.tensor_scalar(out=TL[:], in0=TL[:], scalar1=omlb[:, col:col + 1], scalar2=lbt[:, col:col + 1], op0=ALU.mult, op1=ALU.add), [rTL, r_lb], [rTL])
                                kb.op("act", lambda e: e.activation(out=tg[:], in_=TL[:], func=AF.Ln), [rTL], [r_tg])
                                kb.op("dve", lambda e: e.tensor_tensor_scan(out=ta[:], data0=rmask[:], data1=tg[:], initial=0.0, op0=ALU.mult, op1=ALU.add), [r_rm, r_tg], [r_ta])
                                tav = ta[:].rearrange("p (c t) -> p c t", t=64)
                                if d == 0:
                                    A, rA = ta, r_ta
                                else:
                                    kb.op("dve", lambda e: e.tensor_tensor(out=tb_[:], in0=tg[:], in1=ta[:], op=ALU.subtract), [r_tg, r_ta], [r_tb])
                                    tbv = tb_[:].rearrange("p (c t) -> p c t", t=64)
                                    kb.op("dve", lambda e: e.tensor_tensor(out=tbv, in0=tbv, in1=tav[:, :, 63:64].broadcast_to([128, CG, 64]), op=ALU.add), [r_tb, r_ta], [r_tb])
                                    A, rA = tb_, r_tb
                                Av = A[:].rearrange("p (c t) -> p c t", t=64)
                                mid = Av[:, :, 32]
                                tot = Av[:, :, 63] if d == 0 else Av[:, :, 0]
                                c0 = g * CG
                                kb.op("act", lambda e: e.activation(out=ev[:, 0, d, fc, c0:c0 + CG], in_=mid, func=AF.Exp), [rA], [r_ev])
                                kb.op("act", lambda e: e.activation(out=ev[:, 1, d, fc, c0:c0 + CG], in_=tot, func=AF.Exp), [rA], [r_ev])
                                kb.op("dve", lambda e: e.tensor_tensor(out=t16[:], in0=tot, in1=mid, op=ALU.subtract), [rA], [r_t16])
                                kb.op("act", lambda e: e.activation(out=ev[:, 2, d, fc, c0:c0 + CG], in_=t16[:], func=AF.Exp), [r_t16], [r_ev])
                                tcv = tc_[:].rearrange("p (c t) -> p c t", t=64)
                                kb.op("dve", lambda e: e.tensor_tensor(out=tcv, in0=Av, in1=Av[:, :, 32:33].broadcast_to([128, CG, 64]), op=ALU.subtract), [rA], [r_tc])
                                kb.op("dve", lambda e: e.tensor_scalar(out=tc_[:], in0=tc_[:], scalar1=-40.0, scalar2=40.0, op0=ALU.max, op1=ALU.min), [r_tc], [r_tc])
                                kb.op("act", lambda e: e.activation(out=te[:], in_=tc_[:], func=AF.Exp), [r_tc], [r_te])
                                kb.op("dve", lambda e: e.tensor_tensor(out=OQ[:], in0=Q[:, fc, :], in1=te[:], op=ALU.mult), [rQ, r_te], [rOQ])
                                kb.op("act", lambda e: e.activation(out=te[:], in_=tc_[:], func=AF.Exp, scale=-1.0), [r_tc, r_te], [r_te])
                                kb.op("dve", lambda e: e.tensor_scalar(out=TL[:], in0=TL[:], scalar1=-1.0, scalar2=1.0, op0=ALU.mult, op1=ALU.add), [rTL], [rTL])
                                kb.op("dve", lambda e: e.tensor_tensor(out=OK[:], in0=TL[:], in1=te[:], op=ALU.mult), [rTL, r_te], [rOK])
                                kb.dma("pool", hqk_t.ap()[d, 0, fc * 128:(fc + 1) * 128, g0:g0 + GT], OQ[:], [rOQ], [r_hqk[2 * g]])
                                kb.dma("pool", hqk_t.ap()[d, 1, fc * 128:(fc + 1) * 128, g0:g0 + GT], OK[:], [rOK], [r_hqk[2 * g + 1]])
                    kb.barrier()
                if hg_stop == 1:
                    return
                Sp = esl.enter_context(SBT("h_Sp", [128, 2, 64, 2, 128], BF16))
                r_Sp = kb.res("h_Sp")
                hall = r_hqk + r_hiz
                with contextlib.ExitStack() as es2:
                    St = es2.enter_context(SBT("h_S", [128, 2, 2, 128], F32))
                    r_St = [[kb.res("h_S%d%d" % (d, fc)) for fc in range(2)] for d in range(2)]
                    bmk = es2.enter_context(SBT("h_bm", [128, 128], F32))
                    r_bmk = kb.res("h_bm", kb.dkey("h_bm"))
                    kb.dma("sync", bmk[:], bmask_t.ap(), [], [r_bmk])
                    for d in range(2):
                        for fc in range(2):
                            kb.op("dve", lambda e, d=d, fc=fc: e.memset(St[:, d, fc, :], 0.0), [], [r_St[d][fc]])
                    kt = [[es2.enter_context(SBT("h_kt%d%d" % (d, i), [128, 2, TB], BF16)) for i in range(2)] for d in range(2)]
                    r_kt = [[kb.res("h_kt%d%d" % (d, i), kb.dkey("h_kt")) for i in range(2)] for d in range(2)]
                    ti = [[es2.enter_context(SBT("h_i%d%d" % (d, i), [64, 8, 256], BF16)) for i in range(2)] for d in range(2)]
                    r_ti = [[kb.res("h_i%d%d" % (d, i), kb.dkey("h_i")) for i in range(2)] for d in range(2)]
                    ktm = [es2.enter_context(SBT("h_ktm%d" % d, [64, 2, 8, 128], BF16)) for d in range(2)]
                    r_ktm = [kb.res("h_ktm%d" % d) for d in range(2)]
                    t1 = [[es2.enter_context(SBT("h_t1%d%d" % (d, fc), [128, 128], F32)) for fc in range(2)] for d in range(2)]
                    r_t1 = [[kb.res("h_t1%d%d" % (d, fc)) for fc in range(2)] for d in range(2)]

                    def p1_load(d, step):
                        grp = step if d == 0 else NTB - 1 - step
                        t0 = grp * TB
                        kb.dma("sync", kt[d][step % 2][:], hqk_t.ap()[d, 1].rearrange("(b p) t -> p b t", p=128)[:, :, t0:t0 + TB], hall, [r_kt[d][step % 2]])
                        kb.dma("sync", ti[d][step % 2][:], hiz_t.ap()[t0:t0 + TB, 0:256].rearrange("(c p) v -> p c v", p=64), hall, [r_ti[d][step % 2]])

                    for d in range(2):
                        p1_load(d, 0)
                    for step in range(NTB):
                        for d in range(2):
                            if step + 1 < NTB:
                                p1_load(d, step + 1)
                            grp = step if d == 0 else NTB - 1 - step
                            KT, rKT = kt[d][step % 2], r_kt[d][step % 2]
                            TI, rTI = ti[d][step % 2], r_ti[d][step % 2]
                            for fc in range(2):
                                for c in range(8):
                                    kb.op("pe", lambda e, fc=fc, c=c: e.transpose(out=pstb[fc][0:64, c * 128:(c + 1) * 128], in_=KT[:, fc, c * 64:(c + 1) * 64], identity=ident[:]),
                                          [rKT, r_ident], [r_pstb[fc]], acc="pe")
                                evac(ktm[d][:, fc, :, :], pstb[fc][0:64, :].rearrange("p (c k) -> p c k", k=128), [r_pstb[fc]], [r_ktm[d]])
                            corder = range(8) if d == 0 else range(7, -1, -1)
                            for c in corder:
                                cg = grp * 8 + c
                                for fc in range(2):
                                    p, rp = next_ps()
                                    kb.op("pe", lambda e, fc=fc, c=c: e.matmul(p[:, 0:128], lhsT=ktm[d][:, fc, c, :], rhs=TI[:, c, fc * 128:(fc + 1) * 128], start=True, stop=True),
                                          [r_ktm[d], rTI], [rp])
                                    kb.op("dve", lambda e, fc=fc: e.scalar_tensor_tensor(out=t1[d][fc][:], in0=p[:, 0:128], scalar=ev[:, 2, d, fc, cg:cg + 1], in1=bmk[:], op0=ALU.mult, op1=ALU.mult),
                                          [rp, r_ev, r_bmk], [r_t1[d][fc]])
                                    kb.op("dve", lambda e, fc=fc: e.tensor_scalar_mul(out=Sp[:, d, cg, fc, :], in0=St[:, d, fc, :], scalar1=ev[:, 0, d, fc, cg:cg + 1]),
                                          [r_St[d][fc], r_ev], [r_Sp])
                                    kb.op("dve", lambda e, fc=fc: e.scalar_tensor_tensor(out=St[:, d, fc, :], in0=St[:, d, fc, :], scalar=ev[:, 1, d, fc, cg:cg + 1], in1=t1[d][fc][:], op0=ALU.mult, op1=ALU.add),
                                          [r_St[d][fc], r_ev, r_t1[d][fc]], [r_St[d][fc]])
                    kb.barrier()
                if hg_stop == 2:
                    return
                with contextlib.ExitStack() as es3:
                    trit = es3.enter_context(SBT("h_tri", [64, 2, 64], BF16))
                    r_trit = kb.res("h_tri", kb.dkey("h_tri"))
                    kb.dma("sync", trit[:], tri_t.ap(), [], [r_trit])
                    orow = es3.enter_context(SBT("h_orow", [1, 64], F32))
                    r_orow = kb.res("h_orow", kb.dkey("h_orow"))
                    onb = es3.enter_context(SBT("h_onb", [64, 64], F32))
                    r_onb = kb.res("h_onb")
                    kb.dma("sync", orow[:], bro_t.ap()[l:l + 1, 0:64], [], [r_orow])
                    kb.op("pe", lambda e: e.matmul(ps[0][0:64, 0:64], lhsT=ones[0:1, 0:64], rhs=orow[:], start=True, stop=True), [r_ones, r_orow], [r_ps[0]])
                    kb.op("dve", lambda e: e.tensor_copy(out=onb[:], in_=ps[0][0:64, 0:64]), [r_ps[0]], [r_onb])
                    if hg_stop == 3:
                        kb.barrier()
                        return
                    qk = [es3.enter_context(SBT("h_qk%d" % i, [128, 2, 2, 2, TB], BF16)) for i in range(2)]
                    r_qk = [kb.res("h_qk%d" % i, kb.dkey("h_qk")) for i in range(2)]
                    tiz = [es3.enter_context(SBT("h_iz%d" % i, [64, 8, 512], BF16)) for i in range(2)]
                    r_tiz = [kb.res("h_iz%d" % i, kb.dkey("h_iz")) for i in range(2)]
                    Pm = [es3.enter_context(SBT("h_pm%d" % i, [64, 2, 4, 64], BF16)) for i in range(2)]
                    r_Pm = [kb.res("h_pm%d" % i) for i in range(2)]
                    osb = es3.enter_context(SBT("h_osb", [64, 8, 256], F32))
                    r_osb = kb.res("h_osb")
                    osq = es3.enter_context(SBT("h_osq", [64, 8, 256], F32))
                    r_osq = kb.res("h_osq")
                    ssq = es3.enter_context(SBT("h_ssq", [64, 32], F32))
                    r_ssq = kb.res("h_ssq")
                    ym = es3.enter_context(SBT("h_ym", [64, 8, 256], BF16))
                    r_ym = kb.res("h_ym")
                    yo = [es3.enter_context(SBT("h_yo%d" % i, [128, 2, TB], BF16)) for i in range(2)]
                    r_yo = [kb.res("h_yo%d" % i) for i in range(2)]

                    def p2_load(g):
                        t0 = g * TB
                        for d in range(2):
                            for x in range(2):
                                kb.dma("sync", qk[g % 2][:, d, x, :, :], hqk_t.ap()[d, x].rearrange("(b p) t -> p b t", p=128)[:, :, t0:t0 + TB], hall, [r_qk[g % 2]])
                        kb.dma("sync", tiz[g % 2][:], hiz_t.ap()[t0:t0 + TB, :].rearrange("(c p) v -> p c v", p=64), hall, [r_tiz[g % 2]])

                    p2_load(0)
                    ci = 0
                    for g in range(NTB):
                        if g + 1 < NTB:
                            p2_load(g + 1)
                        QK, rQK = qk[g % 2], r_qk[g % 2]
                        IZ, rIZ = tiz[g % 2], r_tiz[g % 2]
                        def emit_scores(c):
                            for hl in range(2):
                                for d in range(2):
                                    for fc in range(2):
                                        kb.op("pe", lambda e, d=d, fc=fc, hl=hl: e.matmul(ps[hl][0:64, (d * 2 + fc) * 64:(d * 2 + fc + 1) * 64], lhsT=QK[hl * 64:(hl + 1) * 64, d, 1, fc, c * 64:(c + 1) * 64],
                                                                                         rhs=QK[hl * 64:(hl + 1) * 64, d, 0, fc, c * 64:(c + 1) * 64], start=True, stop=True),
                                              [rQK], [r_ps[hl]], acc="pe")
                            PMn, rPMn = Pm[c % 2], r_Pm[c % 2]
                            for hl in range(2):
                                kb.op("dve", lambda e, hl=hl: e.tensor_tensor(out=PMn[:, :, hl::2, :], in0=ps[hl][0:64, 0:256].rearrange("p (d f t) -> p d f t", d=2, f=2), in1=trit[:].unsqueeze(2).broadcast_to([64, 2, 2, 64]), op=ALU.mult),
                                      [r_ps[hl], r_trit], [rPMn])

                        emit_scores(0)
                        for c in range(8):
                            cg = g * 8 + c
                            op_, rop = ps[2 + c % 2], r_ps[2 + c % 2]
                            PM, rPM = Pm[c % 2], r_Pm[c % 2]
                            if c + 1 < 8:
                                emit_scores(c + 1)
                            for h in range(4):
                                fc, hl = h // 2, h % 2
                                oo = op_[0:64, h * 64:(h + 1) * 64]
                                kb.op("pe", lambda e, h=h: e.matmul(oo, lhsT=PM[:, 0, h, :], rhs=IZ[:, c, h * 64:(h + 1) * 64], start=True, stop=False), [rPM, rIZ], [rop], acc=("pe" if h > 0 else None))
                                kb.op("pe", lambda e, h=h: e.matmul(oo, lhsT=PM[:, 1, h, :], rhs=IZ[:, c, h * 64:(h + 1) * 64], start=False, stop=False), [rPM, rIZ], [rop], acc="pe")
                                for d in range(2):
                                    kb.op("pe", lambda e, d=d, fc=fc, hl=hl: e.matmul(oo, lhsT=QK[:, d, 0, fc, c * 64:(c + 1) * 64], rhs=Sp[:, d, cg, fc, hl * 64:(hl + 1) * 64], start=False, stop=(d == 1)),
                                          [rQK, r_Sp], [rop], acc="pe")
                            kb.op("act", lambda e: e.copy(out=osb[:, c, :], in_=op_[0:64, 0:256]), [rop], [r_osb])
                        kb.op("act", lambda e: e.activation(out=osq[:], in_=osb[:], func=AF.Square), [r_osb], [r_osq])
                        kb.op("dve", lambda e: e.tensor_reduce(out=ssq[:], in_=osq[:].rearrange("p c (h v) -> p (c h) v", v=64), axis=AX.X, op=ALU.add), [r_osq], [r_ssq])
                        kb.op("act", lambda e: e.activation(out=ssq[:], in_=ssq[:], func=AF.Sqrt, bias=epsc[0:64, :], scale=1.0 / 64), [r_ssq, r_eps], [r_ssq])
                        kb.op("dve", lambda e: e.reciprocal(out=ssq[:], in_=ssq[:]), [r_ssq], [r_ssq])
                        ov3 = osb[:].rearrange("p c (h v) -> p (c h) v", v=64)
                        kb.op("dve", lambda e: e.tensor_tensor(out=ov3, in0=ov3, in1=ssq[:].unsqueeze(2).broadcast_to([64, 32, 64]), op=ALU.mult), [r_osb, r_ssq], [r_osb])
                        kb.op("dve", lambda e: e.tensor_tensor(out=ov3, in0=ov3, in1=onb[:].unsqueeze(1).broadcast_to([64, 32, 64]), op=ALU.mult), [r_osb, r_onb], [r_osb])
                        kb.op("act", lambda e: e.activation(out=osq[:], in_=IZ[:, :, 256:512], func=AF.Silu), [rIZ, r_osq], [r_osq])
                        kb.op("dve", lambda e: e.tensor_tensor(out=ym[:], in0=osb[:], in1=osq[:], op=ALU.mult), [r_osb, r_osq], [r_ym])
                        if hg_stop == 5:
                            continue
                        Y, rY = yo[g % 2], r_yo[g % 2]
                        for half in range(2):
                            for c in range(8):
                                kb.op("pe", lambda e, half=half, c=c: e.transpose(out=pstb[half][:, c * 64:(c + 1) * 64], in_=ym[:, c, half * 128:(half + 1) * 128], identity=ident[0:64, 0:64]),
                                      [r_ym, r_ident], [r_pstb[half]], acc="pe")
                            evac(Y[:, half, :], pstb[half][:, 0:TB], [r_pstb[half]], [rY])
                        kb.dma("pool", yTv[0][:, :, g * TB:(g + 1) * TB], Y[:], [rY], [r_yT[0][g]])
                    kb.barrier()

        def phase_B(l):
            if "sc" in mixers:
                B_sc(l)
            if "conf" in mixers:
                B_conf(l)
            if "mem" in mixers:
                B_mem(l)
            if "attn" in mixers:
                B_attn(l)
            if "hg" in mixers:
                B_hg(l)

        def phase_C(l):
            kb.new_epoch()
            with contextlib.ExitStack() as esl:
                hT = esl.enter_context(SBT("C_h", [128, 8, S], BF16))
                r_h = kb.res("C_h", kb.dkey("C_h"))
                yT = esl.enter_context(SBT("C_y", [128, 10, S], BF16))
                r_y = kb.res("C_y", kb.dkey("C_y"))
                hv = hT_t.ap().rearrange("(k p) t -> p k t", p=128)
                for k in range(8):
                    kb.dma("sync", hT[:, k, :], hv[:, k, :], r_hT, [r_h])
                yv = yT_t.ap().rearrange("n (k p) t -> p (n k) t", p=128)
                for n in range(5):
                    kb.dma("sync", yT[:, 2 * n:2 * n + 2, :], yv[:, 2 * n:2 * n + 2, :], r_yT[n], [r_y])
                wgb = [esl.enter_context(SBT("C_wg%d" % i, [128, 5, 8, 128], BF16)) for i in range(2)]
                wbb = [esl.enter_context(SBT("C_wb%d" % i, [128, 5, 2, 128], BF16)) for i in range(2)]
                r_wd = [kb.res("C_w%d" % i) for i in range(2)]
                sg = [esl.enter_context(SBT("C_sg%d" % i, [128, TB], BF16)) for i in range(3)]
                r_sg = [kb.res("C_sg%d" % i) for i in range(3)]
                tn = [esl.enter_context(SBT("C_tn%d" % i, [128, TB], BF16)) for i in range(3)]
                r_tn = [kb.res("C_tn%d" % i) for i in range(3)]
                mo = [esl.enter_context(SBT("C_mo%d" % i, [128, TB], BF16)) for i in range(2)]
                r_mo = [kb.res("C_mo%d" % i) for i in range(2)]
                stg_g = [esl.enter_context(SBT("C_sgt%d" % i, [128, 8, 128], F32)) for i in range(2)]
                r_stg_g = [kb.res("C_sgt%d" % i, kb.dkey("C_sgt")) for i in range(2)]
                stg_b = [esl.enter_context(SBT("C_sbt%d" % i, [128, 2, 128], F32)) for i in range(2)]
                r_stg_b = [kb.res("C_sbt%d" % i, kb.dkey("C_sbt")) for i in range(2)]
                si = [0]

                def load_d(d):
                    wg = wgb[d % 2]
                    wb = wbb[d % 2]
                    rw = r_wd[d % 2]
                    for n in range(5):
                        i = si[0] % 2
                        si[0] += 1
                        kb.dma("sync", stg_g[i][:], wg_t.ap()[l, n].rearrange("(k p) c -> p k c", p=128)[:, :, d * 128:(d + 1) * 128], [], [r_stg_g[i]])
                        kb.dma("sync", stg_b[i][:], wb_t.ap()[l, n].rearrange("(k p) c -> p k c", p=128)[:, :, d * 128:(d + 1) * 128], [], [r_stg_b[i]])
                        cast(wg[:, n, :, :], stg_g[i][:], [r_stg_g[i]], [rw])
                        cast(wb[:, n, :, :], stg_b[i][:], [r_stg_b[i]], [rw])

                load_d(0)
                mgv = mg_t.ap().rearrange("(k p) t -> p k t", p=128)
                accs = [(pacc, r_pacc), (pss, r_pss)]

                def emit_ident(d, tb, n, j):
                    A, rA = accs[(d * NTB + tb) % 2]
                    kb.op("pe", lambda e: e.matmul(A[:], lhsT=ident[:], rhs=tn[j][:], start=(n == 0), stop=(n == 4)),
                          [r_ident, r_tn[j]], [rA], acc=("pe" if n > 0 else None))
                    if n == 4:
                        m = (d * NTB + tb) % 2
                        evac(mo[m][:], A[:], [rA], [r_mo[m]])
                        kb.dma("pool", mgv[:, d, tb * TB:(tb + 1) * TB], mo[m][:], [r_mo[m]], [r_mg[tb]])

                pend = None
                idx = 0
                for d in range(8):
                    if d + 1 < 8:
                        load_d(d + 1)
                    wg = wgb[d % 2]
                    wb = wbb[d % 2]
                    rw = r_wd[d % 2]
                    for tb in range(NTB):
                        t0 = tb * TB
                        for n in range(5):
                            j = idx % 3
                            idx += 1
                            pG, rG = next_ps()
                            for k in range(8):
                                kb.op("pe", lambda e, k=k, n=n: e.matmul(pG[:], lhsT=wg[:, n, k, :], rhs=hT[:, k, t0:t0 + TB], start=(k == 0), stop=(k == 7)),
                                      [rw, r_h], [rG], acc="pe")
                            kb.op("act", lambda e, j=j: e.activation(out=sg[j][:], in_=pG[:], func=AF.Sigmoid), [rG], [r_sg[j]])
                            pP, rP = next_ps()
                            for k in range(2):
                                kb.op("pe", lambda e, k=k, n=n: e.matmul(pP[:], lhsT=wb[:, n, k, :], rhs=yT[:, 2 * n + k, t0:t0 + TB], start=(k == 0), stop=(k == 1)),
                                      [rw, r_y], [rP], acc="pe")
                            kb.op("dve", lambda e, j=j: e.tensor_tensor(out=tn[j][:], in0=pP[:], in1=sg[j][:], op=ALU.mult), [rP, r_sg[j]], [r_tn[j]])
                            if pend is not None:
                                emit_ident(*pend)
                            pend = (d, tb, n, j)
                emit_ident(*pend)
                kb.barrier()

        def phase_D(l):
            kb.new_epoch()
            src_t = xT_t if l == 0 else out_t
            with contextlib.ExitStack() as esl:
                wsb = esl.enter_context(SBT("D_w", [128, 8, D], BF16))
                r_w = kb.res("D_w")
                with contextlib.ExitStack() as esw:
                    load_w_bf16(esw, "D", wsb, lambda c0, c1: wo_t.ap()[l].rearrange("(k p) c -> p k c", p=128)[:, :, c0:c1], 8, D, r_w)
                    kb.barrier()
                xt = [esl.enter_context(SBT("D_x%d" % i, [128, 8, TB], F32)) for i in range(2)]
                r_xt = [kb.res("D_x%d" % i, kb.dkey("D_x")) for i in range(2)]
                mt = [esl.enter_context(SBT("D_m%d" % i, [128, 8, TB], BF16)) for i in range(2)]
                r_mt = [kb.res("D_m%d" % i, kb.dkey("D_m")) for i in range(2)]
                o = esl.enter_context(SBT("D_o", [128, 8, TB], F32))
                r_o = kb.res("D_o")
                sq = esl.enter_context(SBT("D_sq", [128, 8, TB], F32))
                r_sq = kb.res("D_sq")
                rstd = esl.enter_context(SBT("D_rstd", [128, TB], F32))
                r_rstd = kb.res("D_rstd")
                xv = src_t.ap().rearrange("(k p) t -> p k t", p=128)
                ov = out_t.ap().rearrange("(k p) t -> p k t", p=128)
                mgv = mg_t.ap().rearrange("(k p) t -> p k t", p=128)

                def loads(tb):
                    t0 = tb * TB
                    kb.dma("sync", xt[tb % 2][:], xv[:, :, t0:t0 + TB], [r_out[tb]], [r_xt[tb % 2]])
                    kb.dma("sync", mt[tb % 2][:], mgv[:, :, t0:t0 + TB], [r_mg[tb]], [r_mt[tb % 2]])

                loads(0)
                for tb in range(NTB):
                    t0 = tb * TB
                    if tb + 1 < NTB:
                        loads(tb + 1)
                    X = xt[tb % 2]
                    rX = r_xt[tb % 2]
                    M = mt[tb % 2]
                    rM = r_mt[tb % 2]
                    for do in range(8):
                        p, rp = next_ps()
                        for k in range(8):
                            kb.op("pe", lambda e, k=k, do=do: e.matmul(p[:], lhsT=wsb[:, k, do * 128:(do + 1) * 128], rhs=M[:, k, :], start=(k == 0), stop=(k == 7)),
                                  [r_w, rM], [rp], acc="pe")
                        evac(o[:, do, :], p[:], [rp], [r_o])
                    kb.op("act", lambda e: e.activation(out=sq[:], in_=o[:], func=AF.Square), [r_o], [r_sq])
                    for k in range(8):
                        kb.op("pe", lambda e, k=k: e.matmul(pss[:], lhsT=ones[:], rhs=sq[:, k, :], start=(k == 0), stop=(k == 7)),
                              [r_ones, r_sq], [r_pss], acc="pe")
                    kb.op("act", lambda e: e.activation(out=rstd[:], in_=pss[:], func=AF.Sqrt, bias=epsc[:], scale=1.0 / D), [r_pss, r_eps], [r_rstd])
                    kb.op("dve", lambda e: e.reciprocal(out=rstd[:], in_=rstd[:]), [r_rstd], [r_rstd])
                    for k in range(8):
                        kb.op("dve", lambda e, k=k: e.scalar_tensor_tensor(out=o[:, k, :], in0=o[:, k, :], scalar=pv[:, l, 8 + k:9 + k], in1=rstd[:], op0=ALU.mult, op1=ALU.mult),
                              [r_o, r_pv, r_rstd], [r_o])
                    kb.op("pool", lambda e: e.tensor_tensor(out=X[:], in0=X[:], in1=o[:], op=ALU.add), [rX, r_o], [rX])
                    kb.dma("pool", ov[:, :, t0:t0 + TB], X[:], [rX], [r_out[tb]])
                kb.barrier()

        def dbg_out(name, src_ap_dram, reads):
            kb.dma("sync", dbg_ts[name].ap(), src_ap_dram, reads, [r_dbg])

        for l in range(nlayers):
            if not skipA:
                phase_A(l)
            if stop == "A":
                break
            if not yT_ext:
                phase_B(l)
            if stop == "B":
                break
            phase_C(l)
            phase_D(l)

        if dbg:
            allr = r_hT + r_hgq + r_hff + r_hiz + r_avz + r_mzz + r_aqT + r_akT + r_mg + sum(r_yT, [])
            srcs = {"hT": hT_t, "hgq": hgq_t, "hff": hff_t, "hiz": hiz_t, "avz": avz_t, "aqT": aqT_t, "mg": mg_t, "yT": yT_t}
            for nm in dbg:
                if nm in srcs:
                    dbg_out(nm, srcs[nm].ap(), allr)
                elif nm.startswith("yT") and len(nm) == 3:
                    dbg_out(nm, yT_t.ap()[int(nm[2])], allr)
        kb.barrier()
    return nc


def host_inputs(inputs):
    f = lambda a: np.ascontiguousarray(np.asarray(a, dtype=np.float32))
    x = f(inputs["x"])
    mem = f(inputs["mem"])
    w_in = f(inputs["w_in"])
    perm = np.arange(INW)
    aq0 = 3072
    hp = np.concatenate([np.arange(0, 64), np.arange(128, 192), np.arange(64, 128), np.arange(192, 256)])
    perm[aq0:aq0 + 256] = aq0 + hp
    w_in_p = np.ascontiguousarray(w_in[:, :, perm])
    pv = np.zeros((L, 128, NPV), np.float32)

    def pk(v, n):
        return np.asarray(v, np.float32).reshape(n, 128).T

    for l in range(L):
        pv[l, :, 0:8] = pk(inputs["norm_pre"][l], 8)
        pv[l, :, 8:16] = pk(inputs["norm_post"][l], 8)
        pv[l, :, 16:24] = pk(inputs["mem_norm"][l], 8)
        dw = np.asarray(inputs["conf_dw_w"][l], np.float32)
        for fc in range(2):
            pv[l, :, 24 + fc * 31:24 + (fc + 1) * 31] = dw[:, fc * 128:(fc + 1) * 128].T
        pv[l, :, 86:88] = pk(inputs["conf_dw_b"][l], 2)
        pv[l, :, 88:90] = pk(inputs["conf_ln_g"][l], 2)
        pv[l, :, 90:92] = pk(inputs["conf_ln_b"][l], 2)
        scw = np.asarray(inputs["sc_w"][l], np.float32)
        for fc in range(2):
            pv[l, :, 92 + fc * 3:92 + (fc + 1) * 3] = scw[:, fc * 128:(fc + 1) * 128].T
    lbl = np.zeros((128, 16), np.float32)
    lg = np.asarray(inputs["hg_lb_logits"], np.float32)
    for l in range(L):
        for d in range(2):
            for fc in range(2):
                lbl[:, l * 4 + d * 2 + fc] = lg[l, d, fc * 128:(fc + 1) * 128]
    bro = np.concatenate([np.asarray(inputs["hg_onorm"], np.float32), np.asarray(inputs["attn_sink"], np.float32)], axis=1)
    c = host_consts()
    shared = {
        "w_in": w_in_p, "w_gate": f(inputs["w_gate"]), "w_branch": f(inputs["w_branch"]),
        "w_out": f(inputs["w_out"]), "w_mkv": f(inputs["w_mem_kv"]), "pv": pv, "lbl": lbl,
        "bro": np.ascontiguousarray(bro), "relb": f(inputs["rel_bias"]).reshape(1, 128),
        "ident": c["ident"], "tri": c["tri"], "blockmask": c["blockmask"], "bmask": c["bmask"], "vmask": c["vmask"],
    }
    per = []
    for b in range(x.shape[0]):
        per.append({"xT": np.ascontiguousarray(x[b].T), "memT": np.ascontiguousarray(mem[b].T)})
    return shared, per


def kernel(**inputs):
    shared, per = host_inputs(inputs)
    nc = build()
    in_maps = [dict(shared, **p) for p in per]
    res = run_bass_kernel_spmd(nc, in_maps, core_ids=list(range(8)))
    outs = [np.asarray(r["out"]).T for r in res.results]
    return np.ascontiguousarray(np.stack(outs, axis=0).astype(np.float32))
```
